# Optimizing a Trainium2 kernel written in Bass

```python
import math
import jax
import jax.numpy as jnp
from jax import lax
import numpy as np

D_MODEL = 1024
BATCH = 8
SEQ = 2048
DEPTH = 1

CHUNK = 64
N_META = 16
D_MIX = D_MODEL
A_WIDTH = D_MIX // 2
A_HEAD_DIM = 64
A_HEADS = A_WIDTH // (2 * A_HEAD_DIM)
B_WIDTH = D_MIX - A_WIDTH
B_KEY_DIM = 128
B_VAL_DIM = 128
B_HEADS = B_WIDTH // B_VAL_DIM
ROPE_THETA = 10000.0
Q_BLOCK = 128
A_QK = A_HEADS * 2 * A_HEAD_DIM
A_V = A_HEADS * 2 * A_HEAD_DIM
B_QF = B_HEADS * B_KEY_DIM
B_IG = B_HEADS * B_VAL_DIM
IN_SPLITS = (A_QK, A_QK, A_V, B_QF, B_QF, B_IG, B_IG)
D_IN = A_QK * 2 + A_V + B_QF * 2 + B_IG * 2
N_EXPERTS = 32
TOP_K = 4
D_EXPERT = D_MODEL
SWIGLU_ALPHA = 1.702
SWIGLU_LIMIT = 7.0
EXPERT_BLOCK = 256
DEEPNORM_ALPHA = (2 * DEPTH) ** 0.25
DEEPNORM_BETA = (8 * DEPTH) ** -0.25
LN_EPS = 1e-5
RMS_EPS = 1e-5

kernel_name = 'hybrid_diffattn_hgrn2_moe_block'


def layer_norm(x, g, b):
    xf = x.astype(jnp.float32)
    mu = jnp.mean(xf, axis=-1, keepdims=True)
    var = jnp.mean(jnp.square(xf - mu), axis=-1, keepdims=True)
    return ((xf - mu) * lax.rsqrt(var + LN_EPS) * g.astype(jnp.float32) + b.astype(jnp.float32)).astype(x.dtype)


def rms_norm(x, g):
    xf = x.astype(jnp.float32)
    ms = jnp.mean(jnp.square(xf), axis=-1, keepdims=True)
    return (xf * lax.rsqrt(ms + RMS_EPS) * g.astype(jnp.float32)).astype(x.dtype)


def chunk_ids(L):
    p = np.arange(L)
    return np.where(p < N_META, 0, 1 + (p - N_META) // CHUNK)


def chunk_end(p, L):
    if p < N_META:
        end = N_META
    else:
        end = N_META + CHUNK * ((p - N_META) // CHUNK + 1)
    return min(end, L)


def rope_tables(L, dim):
    pos = jnp.arange(L, dtype=jnp.float32)
    inv = 1.0 / (ROPE_THETA ** (jnp.arange(0, dim, 2, dtype=jnp.float32) / dim))
    ang = pos[:, None] * inv[None, :]
    ang = jnp.concatenate([ang, ang], axis=-1)
    return jnp.cos(ang), jnp.sin(ang)


def apply_rope(x, cos, sin):
    xf = x.astype(jnp.float32)
    half = xf.shape[-1] // 2
    rot = jnp.concatenate([-xf[..., half:], xf[..., :half]], axis=-1)
    return (xf * cos + rot * sin).astype(x.dtype)


def diff_attention(q, k, v, lam, subln_g, lam_init, cos, sin):
    B, L, _ = q.shape
    q = q.reshape(B, L, A_HEADS, 2, A_HEAD_DIM).transpose(0, 2, 3, 1, 4)
    k = k.reshape(B, L, A_HEADS, 2, A_HEAD_DIM).transpose(0, 2, 3, 1, 4)
    v = v.reshape(B, L, A_HEADS, 2 * A_HEAD_DIM).transpose(0, 2, 1, 3)
    q = apply_rope(q, cos, sin) * (A_HEAD_DIM ** -0.5)
    k = apply_rope(k, cos, sin)
    cid = chunk_ids(L)
    outs = []
    for qs in range(0, L, Q_BLOCK):
        qe = min(qs + Q_BLOCK, L)
        ke = chunk_end(qe - 1, L)
        s = jnp.einsum('bhmqd,bhmkd->bhmqk', q[:, :, :, qs:qe], k[:, :, :, :ke]).astype(jnp.float32)
        mask = jnp.asarray(cid[qs:qe, None] >= cid[None, :ke])
        p = jax.nn.softmax(jnp.where(mask, s, -jnp.inf), axis=-1)
        a = p[:, :, 0] - lam * p[:, :, 1]
        outs.append(jnp.einsum('bhqk,bhkd->bhqd', a.astype(v.dtype), v[:, :, :ke]))
    o = jnp.concatenate(outs, axis=2)
    o = rms_norm(o, subln_g) * (1.0 - lam_init)
    return o.transpose(0, 2, 1, 3).reshape(B, L, A_V)


def hgrn2(q, f, i, g, lb, norm_g):
    B, L, _ = q.shape

    def heads(t, d):
        return t.reshape(B, L, B_HEADS, d).transpose(0, 2, 1, 3).astype(jnp.float32)

    qh = jax.nn.silu(heads(q, B_KEY_DIM)) * (B_KEY_DIM ** -0.5)
    z = heads(f, B_KEY_DIM)
    lbh = lb.astype(jnp.float32).reshape(B_HEADS, 1, B_KEY_DIM)
    log_f = jnp.log(lbh + (1.0 - lbh) * jax.nn.sigmoid(z))
    kh = (1.0 - lbh) * jax.nn.sigmoid(-z)
    vh = heads(i, B_VAL_DIM)
    pad = (-L) % CHUNK
    padw = ((0, 0), (0, 0), (pad, 0), (0, 0))
    qh, kh, vh, log_f = (jnp.pad(t, padw) for t in (qh, kh, vh, log_f))
    n = (L + pad) // CHUNK

    def to_chunks(t):
        return t.reshape(B, B_HEADS, n, CHUNK, t.shape[-1]).transpose(2, 0, 1, 3, 4)

    causal = jnp.tril(jnp.ones((CHUNK, CHUNK), dtype=bool))

    def step(S, inp):
        qc, kc, vc, lfc = inp
        b = jnp.cumsum(lfc, axis=2)
        decay = jnp.exp(jnp.where(causal[:, :, None], b[:, :, :, None, :] - b[:, :, None, :, :], -jnp.inf))
        scores = jnp.einsum('bhtk,bhsk,bhtsk->bhts', qc, kc, decay)
        o = jnp.einsum('bhts,bhsv->bhtv', scores, vc) + jnp.einsum('bhtk,bhkv->bhtv', qc * jnp.exp(b), S)
        b_last = b[:, :, -1:, :]
        S = S * jnp.exp(b_last[:, :, 0, :, None]) + jnp.einsum('bhsk,bhsv->bhkv', kc * jnp.exp(b_last - b), vc)
        return S, o

    S0 = jnp.zeros((B, B_HEADS, B_KEY_DIM, B_VAL_DIM), jnp.float32)
    _, o = lax.scan(step, S0, (to_chunks(qh), to_chunks(kh), to_chunks(vh), to_chunks(log_f)))
    o = o.transpose(1, 2, 0, 3, 4).reshape(B, B_HEADS, n * CHUNK, B_VAL_DIM)[:, :, pad:]
    o = rms_norm(o, norm_g) * jax.nn.silu(heads(g, B_VAL_DIM))
    return o.transpose(0, 2, 1, 3).reshape(B, L, B_WIDTH).astype(q.dtype)


def moe(h, w_router, b_router, w_gu, b_gu, w_dn, b_dn):
    T, D = h.shape
    logits = (h @ w_router).astype(jnp.float32) + b_router.astype(jnp.float32)
    top_val, top_idx = lax.top_k(logits, TOP_K)
    gates = jax.nn.softmax(top_val, axis=-1)
    A = T * TOP_K
    e_flat = top_idx.reshape(A).astype(jnp.int32)
    order = jnp.argsort(e_flat)
    e_sorted = e_flat[order]
    counts = jnp.bincount(e_flat, length=N_EXPERTS)
    padded = ((counts + EXPERT_BLOCK - 1) // EXPERT_BLOCK) * EXPERT_BLOCK
    start_sorted = jnp.cumsum(counts) - counts
    cum_pad = jnp.cumsum(padded)
    start_pad = cum_pad - padded
    dest_sorted = (start_pad[e_sorted] + (jnp.arange(A) - start_sorted[e_sorted])).astype(jnp.int32)
    n_blocks = -(-A // EXPERT_BLOCK) + N_EXPERTS
    P = n_blocks * EXPERT_BLOCK
    row_token = jnp.full((P,), T, dtype=jnp.int32).at[dest_sorted].set((order // TOP_K).astype(jnp.int32))
    h_pad = jnp.concatenate([h, jnp.zeros((1, D), h.dtype)], axis=0)
    x_rows = h_pad[row_token].reshape(n_blocks, EXPERT_BLOCK, D)
    block_expert = jnp.clip(jnp.searchsorted(cum_pad, jnp.arange(n_blocks) * EXPERT_BLOCK, side='right'), 0, N_EXPERTS - 1)

    def expert_block(args):
        xb, e = args
        gu = xb @ w_gu[e] + b_gu[e]
        gate = jnp.minimum(gu[:, 0::2], SWIGLU_LIMIT)
        up = jnp.clip(gu[:, 1::2], -SWIGLU_LIMIT, SWIGLU_LIMIT)
        act = (up + 1.0) * gate * jax.nn.sigmoid(gate * SWIGLU_ALPHA)
        return act @ w_dn[e] + b_dn[e]

    y = lax.map(expert_block, (x_rows, block_expert)).reshape(P, D)
    dest = jnp.zeros((A,), jnp.int32).at[order].set(dest_sorted)
    y_assign = y[dest].reshape(T, TOP_K, D)
    return jnp.einsum('tkd,tk->td', y_assign, gates.astype(y.dtype))


def setup_inputs(seed: int = 0) -> dict:
    key = jax.random.key(seed)
    ks = jax.random.split(key, 24)
    f32 = jnp.float32
    nrm = lambda k, s: jax.random.normal(k, s, f32)
    return {
        'x': nrm(ks[0], (BATCH, SEQ, D_MODEL)),
        'meta_tokens': nrm(ks[1], (N_META, D_MODEL)),
        'ln_emb_g': 1.0 + 0.02 * nrm(ks[2], (D_MODEL,)),
        'ln_emb_b': 0.02 * nrm(ks[3], (D_MODEL,)),
        'w_in': nrm(ks[4], (DEPTH, D_MODEL, D_IN)) * D_MODEL ** -0.5,
        'lambda_q1': 0.1 * nrm(ks[5], (DEPTH, A_HEAD_DIM)),
        'lambda_k1': 0.1 * nrm(ks[6], (DEPTH, A_HEAD_DIM)),
        'lambda_q2': 0.1 * nrm(ks[7], (DEPTH, A_HEAD_DIM)),
        'lambda_k2': 0.1 * nrm(ks[8], (DEPTH, A_HEAD_DIM)),
        'subln_g': 1.0 + 0.02 * nrm(ks[9], (DEPTH, 2 * A_HEAD_DIM)),
        'hgrn_lb_table': 0.5 * nrm(ks[10], (DEPTH + 1, B_QF)),
        'hgrn_norm_g': 1.0 + 0.02 * nrm(ks[11], (DEPTH, B_VAL_DIM)),
        'w_out': nrm(ks[12], (DEPTH, D_MIX, D_MODEL)) * (D_MIX ** -0.5) * DEEPNORM_BETA,
        'ln1_g': 1.0 + 0.02 * nrm(ks[13], (DEPTH, D_MODEL)),
        'ln1_b': 0.02 * nrm(ks[14], (DEPTH, D_MODEL)),
        'w_router': nrm(ks[15], (DEPTH, D_MODEL, N_EXPERTS)) * D_MODEL ** -0.5,
        'b_router': 0.01 * nrm(ks[16], (DEPTH, N_EXPERTS)),
        'w_gate_up': nrm(ks[17], (DEPTH, N_EXPERTS, D_MODEL, 2 * D_EXPERT)) * D_MODEL ** -0.5,
        'b_gate_up': 0.01 * nrm(ks[18], (DEPTH, N_EXPERTS, 2 * D_EXPERT)),
        'w_down': nrm(ks[19], (DEPTH, N_EXPERTS, D_EXPERT, D_MODEL)) * (D_EXPERT ** -0.5) * DEEPNORM_BETA,
        'b_down': 0.01 * nrm(ks[20], (DEPTH, N_EXPERTS, D_MODEL)),
        'ln2_g': 1.0 + 0.02 * nrm(ks[21], (DEPTH, D_MODEL)),
        'ln2_b': 0.02 * nrm(ks[22], (DEPTH, D_MODEL)),
    }


def reference(x, meta_tokens, ln_emb_g, ln_emb_b, w_in, lambda_q1, lambda_k1, lambda_q2, lambda_k2,
              subln_g, hgrn_lb_table, hgrn_norm_g, w_out, ln1_g, ln1_b, w_router, b_router,
              w_gate_up, b_gate_up, w_down, b_down, ln2_g, ln2_b):
    B = x.shape[0]
    meta = jnp.broadcast_to(meta_tokens[None].astype(x.dtype), (B, N_META, D_MODEL))
    h = layer_norm(jnp.concatenate([meta, x], axis=1), ln_emb_g, ln_emb_b)
    L = h.shape[1]
    cos, sin = rope_tables(L, A_HEAD_DIM)
    lb_all = jnp.cumsum(jax.nn.softmax(hgrn_lb_table.astype(jnp.float32), axis=0), axis=0)
    split_at = [int(c) for c in np.cumsum(IN_SPLITS)[:-1]]
    for l in range(DEPTH):
        lam_init = 0.8 - 0.6 * math.exp(-0.3 * l)
        proj = jnp.einsum('bld,de->ble', h, w_in[l])
        qa, ka, va, qb, fb, ib, gb = jnp.split(proj, split_at, axis=-1)
        lam = (jnp.exp(jnp.sum(lambda_q1[l].astype(jnp.float32) * lambda_k1[l].astype(jnp.float32)))
               - jnp.exp(jnp.sum(lambda_q2[l].astype(jnp.float32) * lambda_k2[l].astype(jnp.float32)))
               + lam_init)
        ya = diff_attention(qa, ka, va, lam, subln_g[l], lam_init, cos, sin)
        yb = hgrn2(qb, fb, ib, gb, lb_all[l], hgrn_norm_g[l])
        mix = jnp.einsum('ble,ed->bld', jnp.concatenate([ya, yb], axis=-1), w_out[l])
        h = layer_norm(DEEPNORM_ALPHA * h + mix, ln1_g[l], ln1_b[l])
        ffn = moe(h.reshape(-1, D_MODEL), w_router[l], b_router[l], w_gate_up[l], b_gate_up[l],
                  w_down[l], b_down[l]).reshape(h.shape)
        h = layer_norm(DEEPNORM_ALPHA * h + ffn, ln2_g[l], ln2_b[l])
    return h[:, N_META:]
```

```python
import math
import bisect
from contextlib import ExitStack

import numpy as np
import ml_dtypes
import concourse.bass as bass
import concourse.mybir as mybir
from concourse.bass_utils import run_bass_kernel_spmd

F32 = mybir.dt.float32
BF16 = mybir.dt.bfloat16
I32 = mybir.dt.int32
AF = mybir.ActivationFunctionType
ALU = mybir.AluOpType

D = 1024
SEQ = 2048
NT = 17
NMETA = 16
NE = 32
CAP = 448
NS = (CAP + 127) // 128
SROWS = [min(128, CAP - s * 128) for s in range(NS)]
NTRASH = 128
NZ = NE * CAP // 1024
ALPHA = 2.0 ** 0.25
LAM_INIT = 0.2
EPS = 1e-5
DEBUG = False


class Tracker:
    def __init__(self, nc, es, ndma=40, plan=None):
        self.plan = plan
        self.planset = {k: set(v) for k, v in plan.items()} if plan else None
        self.record = {}
        self.nc = nc
        self.E = {"pe": nc.tensor, "act": nc.scalar, "dve": nc.vector, "pool": nc.gpsimd, "sp": nc.sync}
        self.semh = {}
        self.cnt = {}
        for n in self.E:
            self.semh[n] = es.enter_context(nc.semaphore("s_" + n))
            self.cnt[n] = 0
        self.ndma = {"sp": ndma, "pool": 24}
        self.rr = {"sp": 0, "pool": 0}
        for q, n in self.ndma.items():
            for k in range(n):
                self.semh[("d" + q, k)] = es.enter_context(nc.semaphore("s_d%s%d" % (q, k)))
                self.cnt[("d" + q, k)] = 0
        self.waited = {n: {} for n in self.E}
        self.lastw = {}
        self.readers = {}
        self.nins = 0

    def _deps(self, r, w):
        deps = {}

        def add(ev):
            if ev is None:
                return
            k, v = ev
            if deps.get(k, 0) < v:
                deps[k] = v

        for t in r:
            add(self.lastw.get(t))
        for t in w:
            add(self.lastw.get(t))
            for k, v in self.readers.get(t, {}).items():
                add((k, v))
        return deps

    def _wait(self, eng, deps):
        for k, v in deps.items():
            if eng == "pe" and k == "pe":
                continue
            if self.waited[eng].get(k, 0) >= v:
                continue
            if isinstance(k, str):
                self.record.setdefault(k, set()).add(v)
                vv = bisect.bisect_right(self.plan[k], v) if self.plan else v
            else:
                vv = v
            self.E[eng].wait_ge(self.semh[k], vv)
            self.waited[eng][k] = v
            self.nins += 1

    def _commit(self, ev, r, w):
        k, v = ev
        for t in r:
            d = self.readers.setdefault(t, {})
            if d.get(k, 0) < v:
                d[k] = v
        for t in w:
            self.lastw[t] = ev
            self.readers[t] = {}

    def op(self, eng, fn, r=(), w=()):
        self._wait(eng, self._deps(r, w))
        ins = fn(self.E[eng])
        self.cnt[eng] += 1
        if self.planset is None or self.cnt[eng] in self.planset.get(eng, ()):
            ins.then_inc(self.semh[eng], 1)
        self.nins += 1
        self._commit((eng, self.cnt[eng]), r, w)

    def dma(self, q, fn, r=(), w=()):
        k = ("d" + q, self.rr[q])
        self.rr[q] = (self.rr[q] + 1) % self.ndma[q]
        deps = self._deps(r, w)
        if self.cnt[k] > 0 and deps.get(k, 0) < self.cnt[k]:
            deps[k] = self.cnt[k]
        self._wait(q, deps)
        ins = fn(self.E[q])
        self.cnt[k] += 16
        ins.then_inc(self.semh[k], 16)
        self.nins += 1
        self._commit((k, self.cnt[k]), r, w)

    def barrier(self):
        deps = {k: v for k, v in self.cnt.items() if v > 0}
        for eng in self.E:
            self._wait(eng, dict(deps))

    def finish(self, q="sp"):
        deps = {}
        for k in self.cnt:
            if self.cnt[k] > 0:
                deps[k] = self.cnt[k]
        self._wait(q, deps)


def build_nc(n_tiles=NT, do_moe=True, taps=None, tap_tile=1, plan=None, return_plan=False):
    taps = taps or {}
    nc = bass.Bass("TRN2", target_bir_lowering=False)
    es = ExitStack()

    def din(name, shape, dt=F32):
        return nc.dram_tensor(name, list(shape), dt, kind="ExternalInput").ap()

    x = din("x", [SEQ, D])
    meta = din("meta_tokens", [NMETA, D])
    ln_emb_g = din("ln_emb_g", [1, D]); ln_emb_b = din("ln_emb_b", [1, D])
    w_in = din("w_in", [D, 3584])
    lq1 = din("lambda_q1", [1, 64]); lk1 = din("lambda_k1", [1, 64])
    lq2 = din("lambda_q2", [1, 64]); lk2 = din("lambda_k2", [1, 64])
    subln_g = din("subln_g", [1, 128])
    lb_table = din("hgrn_lb_table", [2, 512])
    hg_norm_g = din("hgrn_norm_g", [1, 128])
    w_out = din("w_out", [D, D])
    ln1_g = din("ln1_g", [1, D]); ln1_b = din("ln1_b", [1, D])
    w_router = din("w_router", [D, NE]); b_router = din("b_router", [1, NE])
    w_gu = din("w_gate_up", [NE, D, 2 * D]); b_gu = din("b_gate_up", [NE, 2 * D])
    w_dn = din("w_down", [NE, D, D]); b_dn = din("b_down", [NE, D])
    ln2_g = din("ln2_g", [1, D]); ln2_b = din("ln2_b", [1, D])
    c_identb = din("c_identb", [128, 128], BF16)
    c_identf = din("c_identf", [128, 128])
    c_cs = din("c_cs", [NT, 128, 128])
    c_trim = din("c_trim", [128, 128]); c_sel3 = din("c_sel3", [128, 3])
    c_masku = din("c_masku", [128, 512]); c_valid0 = din("c_valid0", [128, 1])
    c_tris = din("c_tris", [128, 128]); c_ones = din("c_ones", [128, 128])
    c_ebase = din("c_ebase", [128, NE])
    c_piota = din("c_piota", [NE, 128])
    c_trash = din("c_trash", [128, 1])
    c_zeros = din("c_zeros", [1024, D], BF16)

    out = nc.dram_tensor("out", [SEQ, D], F32, kind="ExternalOutput").ap()
    kind_i = "ExternalOutput" if DEBUG else "Internal"
    xs = nc.dram_tensor("xs", [NE * CAP + NTRASH, D], BF16, kind="Internal").ap()
    ys = nc.dram_tensor("ys", [NE * CAP, D], F32, kind="Internal").ap()
    h1d = nc.dram_tensor("h1d", [SEQ, D], F32, kind=kind_i).ap()
    if DEBUG:
        dbg_y = nc.dram_tensor("dbg_y", [NT * 128, D], F32, kind="ExternalOutput").ap()

    T = Tracker(nc, es, plan=plan)
    tap_out = {}
    for nm, (shape, dt) in taps.items():
        tap_out[nm] = nc.dram_tensor("tap_" + nm, list(shape), dt, kind="ExternalOutput").ap()

    def tap(nm, ap, tok, t):
        if nm in tap_out and t == tap_tile:
            T.dma("sp", lambda e: e.dma_start(out=tap_out[nm], in_=ap), r=[tok], w=["tap_" + nm])
    sb_names = [0]

    def sb(shape, dt=F32, name=None):
        sb_names[0] += 1
        return es_cur[0].enter_context(nc.sbuf_tensor(name or "t%d" % sb_names[0], list(shape), dt))

    es_cur = [es]

    PB = [es.enter_context(nc.psum_tensor("pb%d" % i, [128, 512], F32)) for i in range(8)]
    PBn = ["pb%d" % i for i in range(8)]

    def bcast_row(ap_row, n):
        return ap_row.partition_broadcast(128) if False else ap_row.to_broadcast([128, n])

    identb = sb([128, 128], BF16); identf = sb([128, 128])
    valid0 = sb([128, 1])
    ebase = sb([128, NE])
    lam_col = sb([128, 1])
    gates = sb([128, NT, 4]); slots_f = sb([128, NT, 4]); slots_i = sb([128, NT, 4], I32); gslots_i = sb([128, NT, 4], I32)
    trash_col = sb([128, 1])
    bguT = sb([128, 16, NE])
    T.dma("sp", lambda e: e.dma_start(out=identb[:], in_=c_identb), w=["identb"])
    T.dma("sp", lambda e: e.dma_start(out=identf[:], in_=c_identf), w=["identf"])
    T.dma("sp", lambda e: e.dma_start(out=valid0[:], in_=c_valid0), w=["valid0"])
    T.dma("sp", lambda e: e.dma_start(out=ebase[:], in_=c_ebase), w=["ebase"])
    T.dma("sp", lambda e: e.dma_start(out=trash_col[:], in_=c_trash), w=["trash"])

    with ExitStack() as esA:
        es_cur[0] = esA
        w_in_sb = sb([128, 8, 3584], BF16)
        w_out_sb = sb([128, 8, D], BF16)
        w_r_sb = sb([128, 8, NE])
        b_r_b = sb([128, NE])
        kT_all = sb([128, 4, NT * 128], BF16)
        Vaug = sb([128, NT, 4, 130], BF16)
        g_emb = sb([128, D]); b_emb = sb([128, D]); g_1 = sb([128, D]); b_1 = sb([128, D])
        trim = sb([128, 128]); sel3 = sb([128, 3]); masku = sb([128, 512])
        tris = sb([128, 128]); ones = sb([128, 128])
        lb_b = sb([128, 512]); oml_b = sb([128, 512]); ng_b = sb([128, 128]); sg_b = sb([128, 128])
        S_st = sb([128, 4, 128]); cum = sb([128, NE])
        xt = sb([128, D]); hf = sb([128, D]); hT = sb([128, 8, 128], BF16); hf2 = sb([128, D]); hf3 = sb([128, D]); hT2 = sb([128, 8, 128], BF16)
        hb = sb([128, D], BF16); zb = sb([128, D]); cs_t = sb([128, 2, 128])
        st6 = sb([128, 2, 6]); mv = sb([128, 2]); rstd = sb([128, 1]); nmr = sb([128, 1])
        st6b = sb([128, 2, 6]); mvb = sb([128, 2]); rstdb = sb([128, 1]); nmrb = sb([128, 1])
        LNT_A = (st6, mv, rstd, nmr, 'A'); LNT_B = (st6b, mvb, rstdb, nmrb, 'B')
        qr = sb([128, 512], BF16); kr = sb([128, 512], BF16)
        qT = sb([128, 4, 128], BF16)
        PT0 = sb([16, 128], BF16); PTc = [sb([128, 512], BF16), sb([128, 512], BF16)]
        rr_ = sb([128, 2]); t1 = sb([128, 128]); adiff = sb([128, 4, 128]); junk = sb([128, 128]); junk2 = junk
        ss = sb([128, 8]); rs8 = sb([128, 8])
        ybuf = sb([128, D], BF16); yT = sb([128, 8, 128], BF16)
        h_eq = sb([128, 512]); h_qh = sb([128, 512]); h_ef = sb([128, 512]); h_tt = sb([128, 512])
        h_lf = sb([128, 512]); h_kk = sb([128, 512]); h_v = sb([128, 512], BF16)
        h_sgg = sb([128, 512]); h_E = h_eq; tA = h_ef; tB = h_tt
        h_qt = sb([128, 512], BF16); h_kt = sb([128, 512], BF16); qkT = sb([128, 8, 128], BF16)
        dec = sb([128, 12]); Sx = sb([128, 4, 128], BF16); scT = sb([128, 4, 128], BF16)
        o_sb = sb([128, 512]); utmp = sb([128, 4, 128])
        z1 = zb; h1b = sb([128, D], BF16); h1T = zb[:].rearrange("p (a b) -> p a b", b=128)
        lg = sb([128, NE]); top8 = sb([128, 8]); negmx = sb([128, 1]); e4 = sb([128, 4]); den = sb([128, 1])
        maskf = sb([128, NE]); slotf = sb([128, NE]); novf = sb([128, NE]); nk4 = sb([128, 4]); oneh = sb([128, 4, NE]); junk32 = sb([128, NE])

        for blk in (4, 3, 5, 6, 0, 1, 2):
            T.dma("pool", lambda e, blk=blk: e.dma_start(
                out=w_in_sb[:, :, blk * 512:(blk + 1) * 512],
                in_=w_in[:, blk * 512:(blk + 1) * 512].rearrange("(c p) n -> p c n", p=128)), w=["w_in%d" % blk])
        for (dst, src, nm) in ((g_emb, ln_emb_g, "g_emb"), (b_emb, ln_emb_b, "b_emb")):
            T.dma("sp", lambda e, dst=dst, src=src: e.dma_start(out=dst[:], in_=src.to_broadcast([128, D])), w=[nm])
        T.dma("sp", lambda e: e.dma_start(out=cs_t[:, 0, :], in_=c_cs[0]), w=["cs0"])
        T.op("pool", lambda e: e.memset(xt[:], 0.0), w=["xt"])
        T.dma("sp", lambda e: e.dma_start(out=xt[0:NMETA, :], in_=meta), w=["xt"])
        T.dma("sp", lambda e: e.dma_start(out=lb_b[:], in_=lb_table[0:1, :].to_broadcast([128, 512])), w=["lb_b"])
        T.dma("sp", lambda e: e.dma_start(out=oml_b[:], in_=lb_table[1:2, :].to_broadcast([128, 512])), w=["oml_b"])
        for (dst, src, nm) in ((trim, c_trim, "trim"), (sel3, c_sel3, "sel3"), (masku, c_masku, "masku"),
                               (tris, c_tris, "tris"), (ones, c_ones, "ones")):
            T.dma("sp", lambda e, dst=dst, src=src: e.dma_start(out=dst[:], in_=src), w=[nm])
        for (dst, src, nm) in ((g_1, ln1_g, "g_1"), (b_1, ln1_b, "b_1")):
            T.dma("sp", lambda e, dst=dst, src=src: e.dma_start(out=dst[:], in_=src.to_broadcast([128, D])), w=[nm])
        T.dma("sp", lambda e: e.dma_start(out=ng_b[:], in_=hg_norm_g.to_broadcast([128, 128])), w=["ng_b"])
        T.dma("sp", lambda e: e.dma_start(out=sg_b[:], in_=subln_g.to_broadcast([128, 128])), w=["sg_b"])
        T.dma("sp", lambda e: e.dma_start(out=b_r_b[:], in_=b_router.to_broadcast([128, NE])), w=["b_r_b"])
        T.dma("sp", lambda e: e.dma_start(out=w_r_sb[:], in_=w_router.rearrange("(c p) n -> p c n", p=128)), w=["w_r"])
        for hh in range(2):
            T.dma("pool", lambda e, hh=hh: e.dma_start(
                out=w_out_sb[:, :, hh * 512:(hh + 1) * 512],
                in_=w_out[:, hh * 512:(hh + 1) * 512].rearrange("(c p) n -> p c n", p=128)), w=["w_out"])
        lt = sb([128, 4, 64]); ltmp = sb([128, 64]); lsum = sb([128, 2])
        for i, a_ in enumerate((lq1, lk1, lq2, lk2)):
            T.dma("sp", lambda e, i=i, a_=a_: e.dma_start(out=lt[:, i, :], in_=a_.to_broadcast([128, 64])), w=["lt%d" % i])
        for i in range(2):
            T.op("dve", lambda e, i=i: e.scalar_tensor_tensor(
                out=ltmp[:], in0=lt[:, 2 * i, :], scalar=1.0, in1=lt[:, 2 * i + 1, :],
                op0=ALU.mult, op1=ALU.mult, accum_out=lsum[:, i:i + 1]), r=["lt%d" % (2 * i), "lt%d" % (2 * i + 1)], w=["ltmp", "lsum"])
        T.op("act", lambda e: e.activation(out=lsum[:], in_=lsum[:], func=AF.Exp), r=["lsum"], w=["lsum"])
        T.op("dve", lambda e: e.tensor_scalar(out=lam_col[:], in0=lsum[:, 0:1], scalar1=lsum[:, 1:2],
                                              scalar2=LAM_INIT, op0=ALU.subtract, op1=ALU.add),
             r=["lsum"], w=["lam"])
        T.op("dve", lambda e: e.tensor_tensor(out=oml_b[:], in0=oml_b[:], in1=lb_b[:], op=ALU.subtract),
             r=["lb_b"], w=["oml_b"])
        T.op("act", lambda e: e.activation(out=oml_b[:], in_=oml_b[:], func=AF.Exp), w=["oml_b"])
        T.op("dve", lambda e: e.tensor_scalar(out=oml_b[:], in0=oml_b[:], scalar1=1.0, scalar2=None, op0=ALU.add), w=["oml_b"])
        T.op("dve", lambda e: e.reciprocal(out=lb_b[:], in_=oml_b[:]), r=["oml_b"], w=["lb_b"])
        T.op("dve", lambda e: e.tensor_scalar(out=oml_b[:], in0=lb_b[:], scalar1=-1.0, scalar2=1.0, op0=ALU.mult, op1=ALU.add),
             r=["lb_b"], w=["oml_b"])
        T.op("dve", lambda e: e.tensor_scalar(out=sg_b[:], in0=sg_b[:], scalar1=1.0 - LAM_INIT, scalar2=None, op0=ALU.mult), w=["sg_b"])
        tap("lb_b", lb_b[:], "lb_b", tap_tile)
        for blk in range(7):
            tap("w_in%d" % blk, w_in_sb[:, :, blk * 512:(blk + 1) * 512], "w_in%d" % blk, tap_tile)
        tap("oml_b", oml_b[:], "oml_b", tap_tile)
        tap("sg_b", sg_b[:], "sg_b", tap_tile)
        tap("lam", lam_col[:], "lam", tap_tile)
        tap("bguT", bguT[:].rearrange("p a b -> p (a b)"), "bguT", tap_tile)
        T.op("dve", lambda e: e.memset(S_st[:], 0.0), w=["S"])
        T.op("dve", lambda e: e.memset(scT[:], 0.0), w=["scT"])
        T.op("dve", lambda e: e.memset(cum[:], 0.0), w=["cum"])
        T.op("pool", lambda e: e.memset(Vaug[:, :, :, 128:130], 1.0), w=["Vones"])
        T.op("pool", lambda e: e.memset(PTc[0][:], 0.0), w=["PTc0"])
        T.op("pool", lambda e: e.memset(PTc[1][:], 0.0), w=["PTc1"])

        def layer_norm(src_ap, src_tok, g_t, b_t, g_tok, b_tok, dst, dst_tok):
            for hh in range(2):
                T.op("dve", lambda e, hh=hh: e.bn_stats(out=st6[:, hh, :], in_=src_ap[:, hh * 512:(hh + 1) * 512]),
                     r=[src_tok], w=["st6"])
            T.op("dve", lambda e: e.bn_aggr(out=mv[:], in_=st6[:].rearrange("p a b -> p (a b)")), r=["st6"], w=["mv"])
            T.op("act", lambda e: e.activation(out=rstd[:], in_=mv[:, 1:2], func=AF.Ln, bias=eps_col[:], scale=1.0),
                 r=["mv", "eps"], w=["rstd"])
            T.op("act", lambda e: e.activation(out=rstd[:], in_=rstd[:], func=AF.Exp, scale=-0.5), w=["rstd"])
            T.op("dve", lambda e: e.tensor_scalar(out=nmr[:], in0=mv[:, 0:1], scalar1=rstd[:, 0:1], scalar2=-1.0,
                                                  op0=ALU.mult, op1=ALU.mult), r=["mv", "rstd"], w=["nmr"])
            T.op("act", lambda e: e.activation(out=dst[:], in_=src_ap[:], func=AF.Identity, bias=nmr[:, 0:1], scale=rstd[:, 0:1]),
                 r=[src_tok, "rstd", "nmr"], w=[dst_tok])
            T.op("dve", lambda e: e.tensor_tensor(out=dst[:], in0=dst[:], in1=g_t[:], op=ALU.mult), r=[g_tok], w=[dst_tok])
            T.op("dve", lambda e: e.tensor_tensor(out=dst[:], in0=dst[:], in1=b_t[:], op=ALU.add), r=[b_tok], w=[dst_tok])

        eps_col = sb([128, 1]); one_col = sb([128, 1])
        T.op("dve", lambda e: e.memset(eps_col[:], EPS), w=["eps"])
        T.op("dve", lambda e: e.memset(one_col[:], 1.0), w=["one"])

        def transpose8(src, src_tok, dst, dst_tok, n=8, evac="act"):
            pv = PB[2][:].bitcast(BF16)
            for c in range(n):
                T.op("pe", lambda e, c=c: e.transpose(pv[:, c * 128:(c + 1) * 128], src[:, c * 128:(c + 1) * 128], identb[:]),
                     r=[src_tok, "identb"], w=[PBn[2]])
            if evac == "act":
                T.op("act", lambda e: e.activation(out=dst[:].rearrange("p a b -> p (a b)"), in_=pv[:, 0:n * 128], func=AF.Identity),
                     r=[PBn[2]], w=[dst_tok])
            else:
                T.op("dve", lambda e: e.tensor_copy(out=dst[:].rearrange("p a b -> p (a b)"), in_=pv[:, 0:n * 128]),
                     r=[PBn[2]], w=[dst_tok])

        def sigmoid_from_exp(buf, tok):
            T.op("act", lambda e: e.activation(out=buf[:], in_=buf[:], func=AF.Ln, bias=one_col[:], scale=1.0), r=["one"], w=[tok])
            T.op("act", lambda e: e.activation(out=buf[:], in_=buf[:], func=AF.Exp, scale=-1.0), w=[tok])


        hfb = [hf, hf2, hf3]; hTb = [hT, hT2]

        def interleave(gens, est, delay=None):
            n = len(gens)
            delay = list(delay) if delay else [0] * n
            done = [0] * n
            alive = [True] * n
            total = 0
            while any(alive):
                best = None
                for i in range(n):
                    if not alive[i]:
                        continue
                    if delay[i] > total and any(alive[j] and delay[j] <= total for j in range(n)):
                        continue
                    if best is None or done[i] / est[i] < done[best] / est[best]:
                        best = i
                try:
                    next(gens[best])
                    done[best] += 1
                except StopIteration:
                    alive[best] = False
                total += 1

        def F1(t):
            p = t % 2
            if t == 0:
                pass
            else:
                T.dma("sp", lambda e: e.dma_start(out=cs_t[:, p, :], in_=c_cs[t]), w=["cs%d" % p])
                T.dma("sp", lambda e: e.dma_start(out=xt[:], in_=x[(t - 1) * 128:t * 128, :]), w=["xt"])
            yield
            for _ in layer_norm_g(xt, "xt", g_emb, b_emb, "g_emb", "b_emb", hfb[t % 3], "hf%d" % (t % 3), LNT_A):
                yield
            T.op("act", lambda e: e.activation(out=hb[:], in_=hfb[t % 3][:], func=AF.Identity), r=["hf%d" % (t % 3)], w=["hb"])
            yield
            pv = PB[2][:].bitcast(BF16)
            for c in range(8):
                T.op("pe", lambda e, c=c: e.transpose(pv[:, c * 128:(c + 1) * 128], hb[:, c * 128:(c + 1) * 128], identb[:]),
                     r=["hb", "identb"], w=[PBn[2]])
            T.op("act", lambda e: e.activation(out=hTb[p][:].rearrange("p a b -> p (a b)"), in_=pv[:, 0:1024], func=AF.Identity),
                 r=[PBn[2]], w=["hT%d" % p])
            yield

        def layer_norm_g(src_ap, src_tok, g_t, b_t, g_tok, b_tok, dst, dst_tok, tmp):
            st6_, mv_, rstd_, nmr_, sfx = tmp
            for hh in range(2):
                T.op("dve", lambda e, hh=hh: e.bn_stats(out=st6_[:, hh, :], in_=src_ap[:, hh * 512:(hh + 1) * 512]),
                     r=[src_tok], w=["st6" + sfx])
            T.op("dve", lambda e: e.bn_aggr(out=mv_[:], in_=st6_[:].rearrange("p a b -> p (a b)")), r=["st6" + sfx], w=["mv" + sfx])
            yield
            T.op("act", lambda e: e.activation(out=rstd_[:], in_=mv_[:, 1:2], func=AF.Ln, bias=eps_col[:], scale=1.0),
                 r=["mv" + sfx, "eps"], w=["rstd" + sfx])
            T.op("act", lambda e: e.activation(out=rstd_[:], in_=rstd_[:], func=AF.Exp, scale=-0.5), w=["rstd" + sfx])
            yield
            T.op("dve", lambda e: e.tensor_scalar(out=nmr_[:], in0=mv_[:, 0:1], scalar1=rstd_[:, 0:1], scalar2=-1.0,
                                                  op0=ALU.mult, op1=ALU.mult), r=["mv" + sfx, "rstd" + sfx], w=["nmr" + sfx])
            T.op("act", lambda e: e.activation(out=dst[:], in_=src_ap[:], func=AF.Identity, bias=nmr_[:, 0:1], scale=rstd_[:, 0:1]),
                 r=[src_tok, "rstd" + sfx, "nmr" + sfx], w=[dst_tok])
            yield
            T.op("dve", lambda e: e.tensor_tensor(out=dst[:], in0=dst[:], in1=g_t[:], op=ALU.mult), r=[g_tok], w=[dst_tok])
            T.op("pool", lambda e: e.tensor_tensor(out=dst[:], in0=dst[:], in1=b_t[:], op=ALU.add), r=[b_tok], w=[dst_tok])
            yield

        def F2a(t):
            p = t % 2
            hTp = hTb[p]
            hTn = "hT%d" % p
            csn = "cs%d" % p

            pos = [0]

            def inproj(blk):
                bk = pos[0] % 2
                pos[0] += 1
                pb = PB[bk]
                for c in range(8):
                    T.op("pe", lambda e, c=c: e.matmul(pb[:], lhsT=hTp[:, c, :], rhs=w_in_sb[:, c, blk * 512:(blk + 1) * 512],
                                                        start=(c == 0), stop=(c == 7)),
                         r=[hTn, "w_in%d" % blk], w=[PBn[bk]])
                return pb, PBn[bk]

            def rope(pb, pbn, dst, dst_tok):
                pv = pb[:].rearrange("p (a b) -> p a b", b=64)
                pv4 = pb[:].rearrange("p (a h b) -> p a h b", h=2, b=32)
                tB4 = tB[:].rearrange("p (a h b) -> p a h b", h=2, b=32)
                T.op("dve", lambda e: e.tensor_tensor(out=tA[:].rearrange("p (a b) -> p a b", b=64), in0=pv,
                                                      in1=cs_t[:, p:p + 1, 0:64].to_broadcast([128, 8, 64]), op=ALU.mult),
                     r=[pbn, csn], w=["h_ef"])
                T.op("dve", lambda e: e.tensor_tensor(out=tB4[:, :, 0, :], in0=pv4[:, :, 1, :],
                                                      in1=cs_t[:, p:p + 1, 64:96].to_broadcast([128, 8, 32]), op=ALU.mult),
                     r=[pbn, csn], w=["h_tt"])
                T.op("dve", lambda e: e.tensor_tensor(out=tB4[:, :, 1, :], in0=pv4[:, :, 0, :],
                                                      in1=cs_t[:, p:p + 1, 96:128].to_broadcast([128, 8, 32]), op=ALU.mult),
                     r=[pbn, csn], w=["h_tt"])
                T.op("pool", lambda e: e.tensor_tensor(out=dst[:], in0=tA[:], in1=tB[:], op=ALU.add), r=["h_ef", "h_tt"], w=[dst_tok])

            pb, pbn = inproj(0)
            rope(pb, pbn, qr, "qr")
            yield
            pb, pbn = inproj(1)
            rope(pb, pbn, kr, "kr")
            yield
            pv2 = PB[2][:].bitcast(BF16)
            for h in range(4):
                T.op("pe", lambda e, h=h: e.transpose(pv2[:, h * 128:(h + 1) * 128], qr[:, h * 128:(h + 1) * 128], identb[:]),
                     r=["qr", "identb"], w=[PBn[2]])
            for h in range(4):
                T.op("pe", lambda e, h=h: e.transpose(pv2[:, (4 + h) * 128:(5 + h) * 128], kr[:, h * 128:(h + 1) * 128], identb[:]),
                     r=["kr", "identb"], w=[PBn[2]])
            T.op("act", lambda e: e.activation(out=qT[:].rearrange("p a b -> p (a b)"), in_=pv2[:, 0:512], func=AF.Identity),
                 r=[PBn[2]], w=["qT"])
            T.op("act", lambda e: e.activation(out=kT_all[:, :, t * 128:(t + 1) * 128],
                                               in_=pv2[:, 512:1024].rearrange("p (a b) -> p a b", b=128), func=AF.Identity),
                 r=[PBn[2]], w=["kT%d" % t])
            yield
            pb, pbn = inproj(2)
            T.op("act", lambda e: e.activation(out=Vaug[:, t, :, 0:128], in_=pb[:].rearrange("p (a b) -> p a b", b=128), func=AF.Identity),
                 r=[pbn], w=["V%d" % t])
            yield

        def F2b(t):
            p = t % 2
            hTp = hTb[p]
            hTn = "hT%d" % p
            csn = "cs%d" % p

            pos = [0]

            def inproj(blk):
                bk = pos[0] % 2
                pos[0] += 1
                pb = PB[bk]
                for c in range(8):
                    T.op("pe", lambda e, c=c: e.matmul(pb[:], lhsT=hTp[:, c, :], rhs=w_in_sb[:, c, blk * 512:(blk + 1) * 512],
                                                        start=(c == 0), stop=(c == 7)),
                         r=[hTn, "w_in%d" % blk], w=[PBn[bk]])
                return pb, PBn[bk]

            def rope(pb, pbn, dst, dst_tok):
                pv = pb[:].rearrange("p (a b) -> p a b", b=64)
                pv4 = pb[:].rearrange("p (a h b) -> p a h b", h=2, b=32)
                tB4 = tB[:].rearrange("p (a h b) -> p a h b", h=2, b=32)
                T.op("dve", lambda e: e.tensor_tensor(out=tA[:].rearrange("p (a b) -> p a b", b=64), in0=pv,
                                                      in1=cs_t[:, p:p + 1, 0:64].to_broadcast([128, 8, 64]), op=ALU.mult),
                     r=[pbn, csn], w=["h_ef"])
                T.op("dve", lambda e: e.tensor_tensor(out=tB4[:, :, 0, :], in0=pv4[:, :, 1, :],
                                                      in1=cs_t[:, p:p + 1, 64:96].to_broadcast([128, 8, 32]), op=ALU.mult),
                     r=[pbn, csn], w=["h_tt"])
                T.op("dve", lambda e: e.tensor_tensor(out=tB4[:, :, 1, :], in0=pv4[:, :, 0, :],
                                                      in1=cs_t[:, p:p + 1, 96:128].to_broadcast([128, 8, 32]), op=ALU.mult),
                     r=[pbn, csn], w=["h_tt"])
                T.op("pool", lambda e: e.tensor_tensor(out=dst[:], in0=tA[:], in1=tB[:], op=ALU.add), r=["h_ef", "h_tt"], w=[dst_tok])

            def sig_steps(buf, tok):
                T.op("act", lambda e: e.activation(out=buf[:], in_=buf[:], func=AF.Ln, bias=one_col[:], scale=1.0), r=["one"], w=[tok])
                yield
                T.op("act", lambda e: e.activation(out=buf[:], in_=buf[:], func=AF.Exp, scale=-1.0), w=[tok])
                yield

            pb, pbn = inproj(4)
            yield
            T.op("act", lambda e: e.activation(out=h_ef[:], in_=pb[:], func=AF.Exp, scale=-1.0), r=[pbn], w=["h_ef"])
            yield
            yield from sig_steps(h_ef, "h_ef")
            T.op("dve", lambda e: e.tensor_tensor(out=h_tt[:], in0=h_ef[:], in1=oml_b[:], op=ALU.mult), r=["h_ef", "oml_b"], w=["h_tt"])
            yield
            T.op("dve", lambda e: e.tensor_tensor(out=h_ef[:], in0=h_tt[:], in1=lb_b[:], op=ALU.add), r=["h_tt", "lb_b"], w=["h_ef"])
            T.op("pool", lambda e: e.tensor_tensor(out=h_kk[:], in0=oml_b[:], in1=h_tt[:], op=ALU.subtract), r=["h_tt", "oml_b"], w=["h_kk"])
            yield
            T.op("act", lambda e: e.activation(out=h_lf[:], in_=h_ef[:], func=AF.Ln), r=["h_ef"], w=["h_lf"])
            if t == 0:
                T.op("dve", lambda e: e.tensor_scalar(out=h_lf[:], in0=h_lf[:], scalar1=valid0[:, 0:1], scalar2=None, op0=ALU.mult),
                     r=["valid0"], w=["h_lf"])
                T.op("dve", lambda e: e.tensor_scalar(out=h_kk[:], in0=h_kk[:], scalar1=valid0[:, 0:1], scalar2=None, op0=ALU.mult),
                     r=["valid0"], w=["h_kk"])
            yield
            pb, pbn = inproj(3)
            yield
            T.op("act", lambda e: e.activation(out=h_eq[:], in_=pb[:], func=AF.Exp, scale=-1.0), r=[pbn], w=["h_eq"])
            yield
            yield from sig_steps(h_eq, "h_eq")
            T.op("dve", lambda e: e.scalar_tensor_tensor(out=h_qh[:], in0=pb[:], scalar=128.0 ** -0.5, in1=h_eq[:],
                                                         op0=ALU.mult, op1=ALU.mult), r=[pbn, "h_eq"], w=["h_qh"])
            yield
            pb, pbn = inproj(5)
            T.op("act", lambda e: e.activation(out=h_v[:], in_=pb[:], func=AF.Identity), r=[pbn], w=["h_v"])
            yield
            pb, pbn = inproj(6)
            yield
            T.op("act", lambda e: e.activation(out=h_eq[:], in_=pb[:], func=AF.Exp, scale=-1.0), r=[pbn], w=["h_eq"])
            yield
            yield from sig_steps(h_eq, "h_eq")
            T.op("dve", lambda e: e.scalar_tensor_tensor(out=h_sgg[:], in0=pb[:], scalar=1.0, in1=h_eq[:],
                                                         op0=ALU.mult, op1=ALU.mult), r=[pbn, "h_eq"], w=["h_sgg"])
            T.op("pool", lambda e: e.tensor_tensor(out=h_sgg[:].rearrange("p (a b) -> p a b", b=128),
                                                   in0=h_sgg[:].rearrange("p (a b) -> p a b", b=128),
                                                   in1=ng_b[:].rearrange("p (a b) -> p a b", a=1).to_broadcast([128, 4, 128]),
                                                   op=ALU.mult), r=["ng_b"], w=["h_sgg"])
            yield

        def ATT(t):
            nch = t // 4 + 1
            steps = [(h, m, ch) for h in range(4) for m in range(2) for ch in range(nch)]

            def scores(i):
                h, m, ch = steps[i]
                ps = slice(m * 64, (m + 1) * 64)
                spb = PB[3 + (i % 2)]; spn = PBn[3 + (i % 2)]
                ptb = PTc[i % 2]; ptn = "PTc%d" % (i % 2)
                j0 = ch * 4; j1 = min(t, j0 + 3); nj = j1 - j0 + 1
                for jj in range(nj):
                    j = j0 + jj
                    T.op("pe", lambda e, j=j, jj=jj: e.matmul(spb[:, jj * 128:(jj + 1) * 128], lhsT=kT_all[ps, h, j * 128:(j + 1) * 128],
                                                              rhs=qT[ps, h, :], start=True, stop=True), r=["kT%d" % j, "qT"], w=[spn])
                T.op("act", lambda e: e.activation(out=ptb[:, 0:nj * 128], in_=spb[:, 0:nj * 128], func=AF.Exp, scale=0.125), r=[spn], w=[ptn])
                if j1 == t:
                    jj = nj - 1
                    T.op("dve", lambda e: e.memset(ptb[64:128, jj * 128:jj * 128 + 64], 0.0), w=[ptn])

            def pv(i):
                h, m, ch = steps[i]
                acc = PB[5 + (h % 2)]; accn = PBn[5 + (h % 2)]
                accv = acc[:, 0:260].rearrange("p (m c) -> p m c", c=130)
                ptb = PTc[i % 2]; ptn = "PTc%d" % (i % 2)
                j0 = ch * 4; j1 = min(t, j0 + 3); nj = j1 - j0 + 1
                for jj in range(nj):
                    j = j0 + jj
                    if j == 0:
                        T.op("pe", lambda e: e.matmul(accv[:, m, 0:129], lhsT=ptb[0:16, 0:128], rhs=Vaug[0:16, 0, h, 0:129],
                                                      start=True, stop=(t == 0)), r=[ptn, "Vones", "V0"], w=[accn])
                    else:
                        T.op("pe", lambda e, j=j, jj=jj: e.matmul(accv[:, m, 0:129], lhsT=ptb[:, jj * 128:(jj + 1) * 128], rhs=Vaug[:, j, h, 0:129],
                                                                  start=False, stop=(j == t)), r=[ptn, "Vones", "V%d" % j], w=[accn])
                if m == 1 and ch == nch - 1:
                    T.op("dve", lambda e: e.reciprocal(out=rr_[:], in_=accv[:, :, 128]), r=[accn], w=["rr"])
                    T.op("dve", lambda e: e.tensor_scalar(out=rr_[:, 1:2], in0=rr_[:, 1:2], scalar1=lam_col[:, 0:1], scalar2=None, op0=ALU.mult),
                         r=["lam"], w=["rr"])
                    T.op("dve", lambda e: e.tensor_scalar(out=t1[:], in0=accv[:, 1, 0:128], scalar1=rr_[:, 1:2], scalar2=None, op0=ALU.mult),
                         r=[accn, "rr"], w=["t1"])
                    T.op("dve", lambda e: e.scalar_tensor_tensor(out=adiff[:, h, :], in0=accv[:, 0, 0:128], scalar=rr_[:, 0:1],
                                                                 in1=t1[:], op0=ALU.mult, op1=ALU.subtract),
                         r=[accn, "rr", "t1"], w=["adiff"])
                    T.op("dve", lambda e: e.scalar_tensor_tensor(out=junk[:], in0=adiff[:, h, :], scalar=1.0, in1=adiff[:, h, :],
                                                                 op0=ALU.mult, op1=ALU.mult, accum_out=ss[:, h:h + 1]),
                         r=["adiff"], w=["junk", "ss"])

            scores(0)
            for i in range(len(steps)):
                if i + 1 < len(steps):
                    scores(i + 1)
                pv(i)
                yield

        def HG(t):
            T.op("pe", lambda e: e.matmul(PB[3][:], lhsT=trim[:], rhs=h_lf[:], start=True, stop=True), r=["trim", "h_lf"], w=[PBn[3]])
            for h in range(4):
                T.op("pe", lambda e, h=h: e.matmul(PB[4][:, h * 3:(h + 1) * 3], lhsT=h_lf[:, h * 128:(h + 1) * 128], rhs=sel3[:],
                                                   start=True, stop=True), r=["sel3", "h_lf"], w=[PBn[4]])
            yield
            T.op("act", lambda e: e.activation(out=dec[:], in_=PB[4][:, 0:12], func=AF.Exp), r=[PBn[4]], w=["dec"])
            T.op("act", lambda e: e.activation(out=h_E[:], in_=PB[3][:], func=AF.Exp), r=[PBn[3]], w=["h_eq"])
            yield
            T.op("dve", lambda e: e.tensor_tensor(out=h_qt[:], in0=h_qh[:], in1=h_E[:], op=ALU.mult), r=["h_qh", "h_eq"], w=["h_qt"])
            T.op("act", lambda e: e.activation(out=h_E[:], in_=PB[3][:], func=AF.Exp, scale=-1.0), r=[PBn[3]], w=["h_eq"])
            yield
            T.op("dve", lambda e: e.tensor_tensor(out=h_kt[:], in0=h_kk[:], in1=h_E[:], op=ALU.mult), r=["h_kk", "h_eq"], w=["h_kt"])
            yield
            pv2 = PB[2][:].bitcast(BF16)
            for h in range(4):
                T.op("pe", lambda e, h=h: e.transpose(pv2[:, h * 128:(h + 1) * 128], h_qt[:, h * 128:(h + 1) * 128], identb[:]),
                     r=["h_qt", "identb"], w=[PBn[2]])
            for h in range(4):
                T.op("pe", lambda e, h=h: e.transpose(pv2[:, (4 + h) * 128:(5 + h) * 128], h_kt[:, h * 128:(h + 1) * 128], identb[:]),
                     r=["h_kt", "identb"], w=[PBn[2]])
            T.op("act", lambda e: e.activation(out=qkT[:].rearrange("p a b -> p (a b)"), in_=pv2[:, 0:1024], func=AF.Identity),
                 r=[PBn[2]], w=["qkT"])
            yield
            for h in range(4):
                T.op("act", lambda e, h=h: e.activation(out=Sx[:, h, :], in_=S_st[:, h, :], func=AF.Identity, scale=dec[:, 3 * h:3 * h + 1]),
                     r=["S", "dec"], w=["Sx"])
            yield
            for h in range(4):
                T.op("pe", lambda e, h=h: e.matmul(PB[3][0:64, h * 128:(h + 1) * 128], lhsT=qkT[:, 4 + h, 0:64], rhs=qkT[:, h, :], start=True, stop=True),
                     r=["qkT"], w=[PBn[3]])
                T.op("pe", lambda e, h=h: e.matmul(PB[3][64:128, h * 128 + 64:(h + 1) * 128], lhsT=qkT[:, 4 + h, 64:128], rhs=qkT[:, h, 64:128],
                                                   start=True, stop=True), r=["qkT"], w=[PBn[3]])
            mk3 = masku[:].bitcast(I32).rearrange("p (a b) -> p a b", b=128)
            pb3 = PB[3][:].rearrange("p (a b) -> p a b", b=128)
            T.op("dve", lambda e: e.copy_predicated(out=scT[0:64, :, :].rearrange("p a b -> p (a b)"), mask=masku[0:64, :].bitcast(I32), data=PB[3][0:64, :]),
                 r=[PBn[3], "masku"], w=["scT"])
            T.op("dve", lambda e: e.copy_predicated(out=scT[64:128, :, 64:128], mask=mk3[64:128, :, 64:128], data=pb3[64:128, :, 64:128]),
                 r=[PBn[3], "masku"], w=["scT"])
            yield
            for h in range(4):
                T.op("pe", lambda e, h=h: e.matmul(PB[4][:, h * 128:(h + 1) * 128], lhsT=scT[:, h, :], rhs=h_v[:, h * 128:(h + 1) * 128],
                                                   start=True, stop=False), r=["scT", "h_v"], w=[PBn[4]])
                T.op("pe", lambda e, h=h: e.matmul(PB[4][:, h * 128:(h + 1) * 128], lhsT=qkT[:, h, :], rhs=Sx[:, h, :],
                                                   start=False, stop=True), r=["qkT", "Sx"], w=[PBn[4]])
            for h in range(4):
                T.op("pe", lambda e, h=h: e.matmul(PB[3][:, h * 128:(h + 1) * 128], lhsT=h_kt[:, h * 128:(h + 1) * 128], rhs=h_v[:, h * 128:(h + 1) * 128],
                                                   start=True, stop=True), r=["h_kt", "h_v"], w=[PBn[3]])
            yield
            for h in range(4):
                T.op("act", lambda e, h=h: e.activation(out=utmp[:, h, :], in_=PB[3][:, h * 128:(h + 1) * 128], func=AF.Identity,
                                                        scale=dec[:, 3 * h + 2:3 * h + 3]), r=[PBn[3], "dec"], w=["utmp"])
                T.op("dve", lambda e, h=h: e.scalar_tensor_tensor(out=S_st[:, h, :], in0=S_st[:, h, :], scalar=dec[:, 3 * h + 1:3 * h + 2], in1=utmp[:, h, :],
                                                                  op0=ALU.mult, op1=ALU.add), r=["utmp", "dec"], w=["S"])
                if h % 2 == 1:
                    yield
            if t >= 1:
                T.op("act", lambda e: e.activation(out=o_sb[:], in_=PB[4][:], func=AF.Identity), r=[PBn[4]], w=["o_sb"])
                yield
                for h in range(4):
                    T.op("dve", lambda e, h=h: e.scalar_tensor_tensor(out=junk2[:], in0=o_sb[:, h * 128:(h + 1) * 128], scalar=1.0,
                                                                      in1=o_sb[:, h * 128:(h + 1) * 128], op0=ALU.mult, op1=ALU.mult,
                                                                      accum_out=ss[:, 4 + h:5 + h]), r=["o_sb"], w=["junk", "ss"])
                yield
                T.op("pool", lambda e: e.tensor_tensor(out=o_sb[:], in0=o_sb[:], in1=h_sgg[:], op=ALU.mult), r=["h_sgg"], w=["o_sb"])
                yield

        def YB(t):
            p = t % 3
            hfp = hfb[p]; hfn = "hf%d" % p
            T.op("act", lambda e: e.activation(out=rs8[:], in_=ss[:], func=AF.Ln, bias=eps_col[:], scale=1.0 / 128.0), r=["ss", "eps"], w=["rs8"])
            T.op("act", lambda e: e.activation(out=rs8[:], in_=rs8[:], func=AF.Exp, scale=-0.5), w=["rs8"])
            for h in range(4):
                T.op("dve", lambda e, h=h: e.scalar_tensor_tensor(out=ybuf[:, h * 128:(h + 1) * 128], in0=adiff[:, h, :], scalar=rs8[:, h:h + 1],
                                                                  in1=sg_b[:], op0=ALU.mult, op1=ALU.mult), r=["adiff", "rs8", "sg_b"], w=["ybuf"])
                T.op("act", lambda e, h=h: e.activation(out=ybuf[:, 512 + h * 128:512 + (h + 1) * 128], in_=o_sb[:, h * 128:(h + 1) * 128],
                                                        func=AF.Identity, scale=rs8[:, 4 + h:5 + h]), r=["o_sb", "rs8"], w=["ybuf"])
            if DEBUG:
                T.op("pool", lambda e: e.tensor_copy(out=z1[:], in_=ybuf[:]), r=["ybuf"], w=["zb"])
                T.dma("sp", lambda e: e.dma_start(out=dbg_y[t * 128:(t + 1) * 128, :], in_=z1[:]), r=["zb"], w=["dbg_y"])
            yield
            pv = PB[2][:].bitcast(BF16)
            for c in range(8):
                T.op("pe", lambda e, c=c: e.transpose(pv[:, c * 128:(c + 1) * 128], ybuf[:, c * 128:(c + 1) * 128], identb[:]),
                     r=["ybuf", "identb"], w=[PBn[2]])
            T.op("act", lambda e: e.activation(out=yT[:].rearrange("p a b -> p (a b)"), in_=pv[:, 0:1024], func=AF.Identity),
                 r=[PBn[2]], w=["yT"])
            yield
            for hh in range(2):
                for c in range(8):
                    T.op("pe", lambda e, hh=hh, c=c: e.matmul(PB[5 + hh][:], lhsT=yT[:, c, :], rhs=w_out_sb[:, c, hh * 512:(hh + 1) * 512],
                                                              start=(c == 0), stop=(c == 7)), r=["yT", "w_out"], w=[PBn[5 + hh]])
                T.op("dve", lambda e, hh=hh: e.scalar_tensor_tensor(out=z1[:, hh * 512:(hh + 1) * 512], in0=hfp[:, hh * 512:(hh + 1) * 512],
                                                                    scalar=ALPHA, in1=PB[5 + hh][:], op0=ALU.mult, op1=ALU.add),
                     r=[hfn, PBn[5 + hh]], w=["zb"])
                yield
            h1 = hfp
            for _ in layer_norm_g(z1, "zb", g_1, b_1, "g_1", "b_1", h1, hfn, LNT_B):
                yield
            T.dma("sp", lambda e: e.dma_start(out=h1d[(t - 1) * 128:t * 128, :], in_=h1[:]), r=[hfn], w=["h1d"])
            T.op("act", lambda e: e.activation(out=h1b[:], in_=h1[:], func=AF.Identity), r=[hfn], w=["h1b"])
            yield
            for hh in range(2):
                for c4 in range(4):
                    c = hh * 4 + c4
                    T.op("pe", lambda e, hh=hh, c4=c4, c=c: e.transpose(PB[5 + hh][:, c4 * 128:(c4 + 1) * 128], h1[:, c * 128:(c + 1) * 128], identf[:]),
                         r=[hfn, "identf"], w=[PBn[5 + hh]])
                T.op("act", lambda e, hh=hh: e.activation(out=h1T[:, hh * 4:(hh + 1) * 4, :].rearrange("p a b -> p (a b)"), in_=PB[5 + hh][:], func=AF.Identity),
                     r=[PBn[5 + hh]], w=["zb"])
                yield
            for c in range(8):
                T.op("pe", lambda e, c=c: e.matmul(PB[7][:, 0:NE], lhsT=h1T[:, c, :], rhs=w_r_sb[:, c, :], start=(c == 0), stop=(c == 7)),
                     r=["zb", "w_r"], w=[PBn[7]])
            T.op("dve", lambda e: e.tensor_tensor(out=lg[:], in0=PB[7][:, 0:NE], in1=b_r_b[:], op=ALU.add), r=[PBn[7], "b_r_b"], w=["lg"])
            T.op("dve", lambda e: e.max(out=top8[:], in_=lg[:]), r=["lg"], w=["top8"])
            yield
            T.op("dve", lambda e: e.tensor_scalar(out=negmx[:], in0=top8[:, 0:1], scalar1=-1.0, scalar2=None, op0=ALU.mult), r=["top8"], w=["negmx"])
            T.op("act", lambda e: e.activation(out=e4[:], in_=top8[:, 0:4], func=AF.Exp, bias=negmx[:, 0:1], scale=1.0, accum_out=den[:]),
                 r=["top8", "negmx"], w=["e4", "den"])
            T.op("dve", lambda e: e.tensor_scalar(out=maskf[:], in0=lg[:], scalar1=top8[:, 3:4], scalar2=None, op0=ALU.is_ge),
                 r=["lg", "top8"], w=["maskf"])
            yield
            T.op("pe", lambda e: e.matmul(PB[7][:, 64:64 + NE], lhsT=tris[:], rhs=maskf[:], start=True, stop=False), r=["tris", "maskf"], w=[PBn[7]])
            T.op("pe", lambda e: e.matmul(PB[7][:, 64:64 + NE], lhsT=ones[:], rhs=cum[:], start=False, stop=True), r=["ones", "cum"], w=[PBn[7]])
            T.op("dve", lambda e: e.reciprocal(out=den[:], in_=den[:]), r=["den"], w=["den"])
            T.op("dve", lambda e: e.tensor_scalar(out=gates[:, t, :], in0=e4[:], scalar1=den[:, 0:1], scalar2=None, op0=ALU.mult),
                 r=["e4", "den"], w=["gates"])
            yield
            T.op("dve", lambda e: e.tensor_scalar(out=novf[:], in0=PB[7][:, 64:64 + NE], scalar1=float(CAP), scalar2=None, op0=ALU.is_lt),
                 r=[PBn[7]], w=["novf"])
            T.op("dve", lambda e: e.tensor_tensor(out=slotf[:], in0=PB[7][:, 64:64 + NE], in1=ebase[:], op=ALU.add),
                 r=[PBn[7], "ebase"], w=["slotf"])
            T.op("dve", lambda e: e.tensor_tensor(out=cum[:], in0=cum[:], in1=maskf[:], op=ALU.add), r=["maskf"], w=["cum"])
            yield
            T.op("dve", lambda e: e.scalar_tensor_tensor(out=slotf[:], in0=slotf[:], scalar=trash_col[:, 0:1], in1=novf[:], op0=ALU.subtract, op1=ALU.mult),
                 r=["novf", "trash"], w=["slotf"])
            T.op("dve", lambda e: e.tensor_scalar(out=slotf[:], in0=slotf[:], scalar1=trash_col[:, 0:1], scalar2=None, op0=ALU.add), r=["trash"], w=["slotf"])
            yield
            for k in range(4):
                T.op("dve", lambda e, k=k: e.tensor_scalar(out=oneh[:, k, :], in0=lg[:], scalar1=top8[:, k:k + 1], scalar2=None, op0=ALU.is_equal),
                     r=["lg", "top8"], w=["oneh"])
                T.op("dve", lambda e, k=k: e.scalar_tensor_tensor(out=junk32[:], in0=oneh[:, k, :], scalar=1.0, in1=slotf[:], op0=ALU.mult, op1=ALU.mult,
                                                                  accum_out=slots_f[:, t, k:k + 1]), r=["oneh", "slotf"], w=["junk32", "slots_f"])
                T.op("dve", lambda e, k=k: e.scalar_tensor_tensor(out=junk32[:], in0=oneh[:, k, :], scalar=1.0, in1=novf[:], op0=ALU.mult, op1=ALU.mult,
                                                                  accum_out=nk4[:, k:k + 1]), r=["oneh", "novf"], w=["junk32", "nk4"])
                if k % 2 == 1:
                    yield
            T.op("dve", lambda e: e.tensor_scalar(out=slots_f[:, t, :], in0=slots_f[:, t, :], scalar1=0.0, scalar2=float(NE * CAP + NTRASH - 1),
                                                  op0=ALU.max, op1=ALU.min), w=["slots_f"])
            T.op("dve", lambda e: e.tensor_copy(out=slots_i[:, t, :], in_=slots_f[:, t, :]), r=["slots_f"], w=["slots_i"])
            T.op("dve", lambda e: e.tensor_tensor(out=gates[:, t, :], in0=gates[:, t, :], in1=nk4[:], op=ALU.mult), r=["nk4"], w=["gates"])
            T.op("dve", lambda e: e.tensor_tensor(out=slots_f[:, t, :], in0=slots_f[:, t, :], in1=nk4[:], op=ALU.mult), r=["nk4"], w=["slots_f"])
            T.op("dve", lambda e: e.tensor_copy(out=gslots_i[:, t, :], in_=slots_f[:, t, :]), r=["slots_f"], w=["gslots_i"])
            for k in range(4 if do_moe else 0):
                T.dma("pool", lambda e, k=k: e.indirect_dma_start(
                    out=xs, out_offset=bass.IndirectOffsetOnAxis(ap=slots_i[:, t, k:k + 1], axis=0),
                    in_=h1b[:, :], in_offset=None), r=["h1b", "slots_i"] + ["xs_z%d" % i for i in range(NZ)], w=["xs_sc_%d_%d" % (t, k)])
            yield

        def run(g):
            for _ in g:
                pass

        if DEBUG:
            print("phase A sbuf bytes remaining:", nc.sbuf_bytes_remaining)
        run(F1(0))
        run(F2b(0))
        for t in range(-1, n_tiles):
            if t >= 0:
                gens = []; est = []; dl = []
                if t >= 1:
                    gens.append(ATT(t)); est.append(8.0 * (t // 4 + 1)); dl.append(0)
                if t + 1 < n_tiles:
                    gens.append(F2b(t + 1)); est.append(20.0); dl.append(1)
                if gens:
                    interleave(gens, est, dl)
            gens = []; est = []; dl = []
            if t >= 1:
                yb = YB(t)
                next(yb)
            if t + 1 < n_tiles:
                gens.append(F2a(t + 1)); est.append(4.0); dl.append(0)
                gens.append(HG(t + 1)); est.append(15.0); dl.append(0)
            if t >= 1:
                gens.append(yb); est.append(18.0); dl.append(1)
            if t + 2 < n_tiles:
                gens.append(F1(t + 2)); est.append(8.0); dl.append(2)
            if gens:
                interleave(gens, est, dl)
            if t == -1:
                for i in range(NZ):
                    T.dma("pool", lambda e, i=i: e.dma_start(out=xs[i * 1024:(i + 1) * 1024, :], in_=c_zeros), w=["xs_z%d" % i])
    es_cur[0] = es
    T.barrier()

    with ExitStack() as esB:
        es_cur[0] = esB
        if not do_moe:
            NE_run = 0
        else:
            NE_run = NE
        wgu = [sb([128, 8, 2 * D], BF16), sb([128, 8, 2 * D], BF16)]
        wdn = [sb([128, 8, D], BF16), sb([128, 8, D], BF16)]
        bdn_bf = sb([NE, D], BF16); piota = sb([NE, 128]); selE = [sb([NE, 128], BF16), sb([NE, 128], BF16)]
        bguS = sb([128, 8, NE])
        xrows = [sb([128, NS, D], BF16), sb([128, NS, D], BF16)]
        xT = [sb([128, 8, CAP], BF16), sb([128, 8, CAP], BF16)]
        actT = [sb([128, 8, CAP], BF16), sb([128, 8, CAP], BF16)]
        g1 = [sb([128, CAP]), sb([128, CAP])]; u0 = [sb([128, CAP]), sb([128, CAP])]; gs = [sb([128, CAP]), sb([128, CAP])]
        ysb = [sb([128, D]), sb([128, D])]
        SI = 1.0 / 1.702
        bgu_sb = sb([NE, 2 * D])
        T.dma("sp", lambda e: e.dma_start(out=bgu_sb[:], in_=b_gu), w=["bgu_sb"])
        for j2 in range(16):
            j, two = j2 // 2, j2 % 2
            T.op("pe", lambda e, j=j, two=two, j2=j2: e.transpose(
                PB[0][:, j2 * NE:(j2 + 1) * NE], bgu_sb[:, j * 256 + two:(j + 1) * 256:2], identf[0:NE, 0:NE]),
                r=["bgu_sb", "identf"], w=[PBn[0]])
        T.op("dve", lambda e: e.tensor_copy(out=bguT[:].rearrange("p a b -> p (a b)"), in_=PB[0][:, 0:16 * NE]),
             r=[PBn[0]], w=["bguT"])
        T.dma("pool", lambda e: e.dma_start(out=bdn_bf[:], in_=b_dn), w=["bdn_bf"])
        T.dma("sp", lambda e: e.dma_start(out=piota[:], in_=c_piota), w=["piota"])
        T.op("dve", lambda e: e.tensor_scalar(out=bguS[:], in0=bguT[:, 1:16:2, :], scalar1=SI, scalar2=None, op0=ALU.mult), r=["bguT"], w=["bguS"])

        def load_gu(e_):
            b = e_ % 2
            for hh in range(4):
                T.dma("pool", lambda e, hh=hh: e.dma_start(
                    out=wgu[b][:, :, hh * 512:(hh + 1) * 512],
                    in_=w_gu[e_, :, hh * 512:(hh + 1) * 512].rearrange("(c p) n -> p c n", p=128)), w=["wgu%d_%d" % (b, hh)])

        def load_dn(e_):
            b = e_ % 2
            for hh in range(2):
                T.dma("pool", lambda e, hh=hh: e.dma_start(
                    out=wdn[b][:, :, hh * 512:(hh + 1) * 512],
                    in_=w_dn[e_, :, hh * 512:(hh + 1) * 512].rearrange("(c p) n -> p c n", p=128)), w=["wdn%d_%d" % (b, hh)])

        SC_TOK = ["xs_sc_%d_%d" % (t_, k_) for t_ in range(1, n_tiles) for k_ in range(4)] + ["xs_z%d" % i for i in range(NZ)]

        def load_x(e_):
            b = e_ % 2
            nfull = CAP // 128
            T.dma("sp", lambda e: e.dma_start(out=xrows[b][:, 0:nfull, :], in_=xs[e_ * CAP:e_ * CAP + nfull * 128, :].rearrange("(s p) d -> p s d", p=128)),
                  r=SC_TOK, w=["xrows%d" % b])
            if NS > nfull:
                T.dma("sp", lambda e: e.dma_start(out=xrows[b][0:SROWS[-1], nfull, :], in_=xs[e_ * CAP + nfull * 128:(e_ + 1) * CAP, :]),
                      r=SC_TOK, w=["xrows%d_t" % b])

        if do_moe:
            load_gu(0); load_dn(0); load_gu(1); load_dn(1)
            load_x(0)
        for ex in range(NE_run):
            b = ex % 2
            if ex + 1 < NE:
                load_x(ex + 1)
            T.op("dve", lambda e, ex=ex: e.tensor_scalar(out=selE[b][:], in0=piota[:], scalar1=float(ex), scalar2=None, op0=ALU.is_equal),
                 r=["piota"], w=["selE%d" % b])
            for c2 in range(4):
                pbt = PB[2 + (c2 % 2) * 5]
                pbtn = PBn[2 + (c2 % 2) * 5]
                pvb = pbt[:].bitcast(BF16)
                for cc in range(2):
                    c = c2 * 2 + cc
                    for s in range(NS):
                        T.op("pe", lambda e, c=c, cc=cc, s=s, pvb=pvb: e.transpose(
                            pvb[:, cc * CAP + s * 128:cc * CAP + s * 128 + SROWS[s]], xrows[b][0:SROWS[s], s, c * 128:(c + 1) * 128], identb[0:SROWS[s], 0:SROWS[s]]),
                            r=["xrows%d" % b, "xrows%d_t" % b, "identb"], w=[pbtn])
                T.op("act" if c2 % 2 == 0 else "dve",
                     (lambda e, c2=c2, pvb=pvb: e.activation(out=xT[b][:, c2 * 2:c2 * 2 + 2, :].rearrange("p a b -> p (a b)"), in_=pvb[:, 0:2 * CAP], func=AF.Identity))
                     if c2 % 2 == 0 else
                     (lambda e, c2=c2, pvb=pvb: e.tensor_copy(out=xT[b][:, c2 * 2:c2 * 2 + 2, :].rearrange("p a b -> p (a b)"), in_=pvb[:, 0:2 * CAP])),
                     r=[pbtn], w=["xT%d" % b])
            for j in range(8):
                jb = j % 2
                pg = PB[jb * 3]
                pgn = PBn[jb * 3]
                pu = PB[jb * 3 + 1]
                pun = PBn[jb * 3 + 1]
                wtok = "wgu%d_%d" % (b, j // 2)
                for c in range(8):
                    T.op("pe", lambda e, c=c, j=j, pg=pg: e.matmul(pg[:, 0:CAP], lhsT=wgu[b][:, c, j * 256:(j + 1) * 256:2], rhs=xT[b][:, c, :],
                                                                     start=(c == 0), stop=(c == 7)), r=[wtok, "xT%d" % b], w=[pgn])
                for c in range(8):
                    T.op("pe", lambda e, c=c, j=j, pu=pu: e.matmul(pu[:, 0:CAP], lhsT=wgu[b][:, c, j * 256 + 1:(j + 1) * 256:2], rhs=xT[b][:, c, :],
                                                                     start=(c == 0), stop=(c == 7)), r=[wtok, "xT%d" % b], w=[pun])
                T.op("dve", lambda e, j=j, pg=pg, ex=ex, jb=jb: e.tensor_scalar(out=g1[jb][:], in0=pg[:, 0:CAP], scalar1=bguT[:, 2 * j, ex:ex + 1], scalar2=7.0,
                                                                                op0=ALU.add, op1=ALU.min), r=[pgn, "bguT"], w=["g1_%d" % jb])
                T.op("act", lambda e, j=j, pu=pu, ex=ex, jb=jb: e.activation(out=u0[jb][:], in_=pu[:, 0:CAP], func=AF.Identity, bias=bguS[:, j, ex:ex + 1], scale=SI),
                     r=[pun, "bguS"], w=["u0_%d" % jb])
                T.op("act", lambda e, jb=jb: e.activation(out=gs[jb][:], in_=g1[jb][:], func=AF.Silu, scale=1.702), r=["g1_%d" % jb], w=["gs_%d" % jb])
                T.op("dve", lambda e, jb=jb: e.tensor_scalar(out=u0[jb][:], in0=u0[jb][:], scalar1=7.0 * SI, scalar2=-7.0 * SI, op0=ALU.min, op1=ALU.max), w=["u0_%d" % jb])
                T.op("dve", lambda e, j=j, jb=jb: e.scalar_tensor_tensor(out=actT[b][:, j, :], in0=u0[jb][:], scalar=SI, in1=gs[jb][:], op0=ALU.add, op1=ALU.mult),
                     r=["u0_%d" % jb, "gs_%d" % jb], w=["actT%d" % b])
            if ex + 2 < NE:
                load_gu(ex + 2)
            for s in range(NS):
                yb_ = ysb[s % 2]
                ybn = "ysb%d" % (s % 2)
                R = SROWS[s]
                for hh in range(2):
                    py = PB[5 + hh]
                    T.op("pe", lambda e, hh=hh, py=py: e.matmul(py[0:R, :], lhsT=selE[b][:, 0:R], rhs=bdn_bf[:, hh * 512:(hh + 1) * 512], start=True, stop=False),
                         r=["selE%d" % b, "bdn_bf"], w=[PBn[5 + hh]])
                    for j in range(8):
                        T.op("pe", lambda e, j=j, s=s, hh=hh, py=py: e.matmul(py[0:R, :], lhsT=actT[b][:, j, s * 128:s * 128 + R],
                                                                               rhs=wdn[b][:, j, hh * 512:(hh + 1) * 512], start=False, stop=(j == 7)),
                             r=["actT%d" % b, "wdn%d_%d" % (b, hh)], w=[PBn[5 + hh]])
                    T.op("act" if hh == 0 else "dve",
                         (lambda e, hh=hh, py=py, yb_=yb_: e.activation(out=yb_[0:R, hh * 512:(hh + 1) * 512], in_=py[0:R, :], func=AF.Identity)) if hh == 0 else
                         (lambda e, hh=hh, py=py, yb_=yb_: e.tensor_copy(out=yb_[0:R, hh * 512:(hh + 1) * 512], in_=py[0:R, :])),
                         r=[PBn[5 + hh]], w=[ybn])
                T.dma("sp", lambda e, ex=ex, s=s, yb_=yb_: e.dma_start(out=ys[ex * CAP + s * 128:ex * CAP + s * 128 + R, :], in_=yb_[0:R, :]),
                      r=[ybn], w=["ys"])
            if ex + 2 < NE:
                load_dn(ex + 2)
    es_cur[0] = es
    T.barrier()

    with ExitStack() as esC:
        es_cur[0] = esC
        g_2 = sb([128, D]); b_2 = sb([128, D])
        T.dma("sp", lambda e: e.dma_start(out=g_2[:], in_=ln2_g.to_broadcast([128, D])), w=["g_2"])
        T.dma("sp", lambda e: e.dma_start(out=b_2[:], in_=ln2_b.to_broadcast([128, D])), w=["b_2"])
        NPC = 4
        yk = [[sb([128, D]) for _ in range(4)] for _ in range(NPC)]
        h1r = [sb([128, D]) for _ in range(NPC)]
        accf = [sb([128, D]) for _ in range(NPC)]; z2 = [sb([128, D]) for _ in range(NPC)]; of = [sb([128, D]) for _ in range(NPC)]
        st6 = [sb([128, 2, 6]) for _ in range(NPC)]; mv = [sb([128, 2]) for _ in range(NPC)]
        rstd = [sb([128, 1]) for _ in range(NPC)]; nmr = [sb([128, 1]) for _ in range(NPC)]; eps_col = sb([128, 1])
        T.op("dve", lambda e: e.memset(eps_col[:], EPS), w=["eps2"])

        def CT(t):
            p = t % NPC
            P = "_%d" % p
            for k in range(4):
                T.dma("pool", lambda e, k=k: e.indirect_dma_start(
                    out=yk[p][k][:, :], out_offset=None, in_=ys,
                    in_offset=bass.IndirectOffsetOnAxis(ap=gslots_i[:, t, k:k + 1], axis=0)),
                    r=["ys", "gslots_i"], w=["yk%d_%d" % (p, k)])
            T.dma("sp", lambda e: e.dma_start(out=h1r[p][:], in_=h1d[(t - 1) * 128:t * 128, :]), r=["h1d"], w=["h1r" + P])
            yield
            T.op("dve", lambda e: e.tensor_scalar(out=accf[p][:], in0=yk[p][0][:], scalar1=gates[:, t, 0:1], scalar2=None, op0=ALU.mult),
                 r=["yk%d_0" % p, "gates"], w=["accf" + P])
            yield
            for k in range(1, 4):
                T.op("dve", lambda e, k=k: e.scalar_tensor_tensor(out=accf[p][:], in0=yk[p][k][:], scalar=gates[:, t, k:k + 1], in1=accf[p][:],
                                                                  op0=ALU.mult, op1=ALU.add), r=["yk%d_%d" % (p, k), "gates"], w=["accf" + P])
                yield
            T.op("dve", lambda e: e.scalar_tensor_tensor(out=z2[p][:], in0=h1r[p][:], scalar=ALPHA, in1=accf[p][:], op0=ALU.mult, op1=ALU.add),
                 r=["h1r" + P, "accf" + P], w=["z2" + P])
            yield
            for hh in range(2):
                T.op("dve", lambda e, hh=hh: e.bn_stats(out=st6[p][:, hh, :], in_=z2[p][:, hh * 512:(hh + 1) * 512]), r=["z2" + P], w=["st6c" + P])
            T.op("dve", lambda e: e.bn_aggr(out=mv[p][:], in_=st6[p][:].rearrange("p a b -> p (a b)")), r=["st6c" + P], w=["mvc" + P])
            yield
            T.op("act", lambda e: e.activation(out=rstd[p][:], in_=mv[p][:, 1:2], func=AF.Ln, bias=eps_col[:], scale=1.0), r=["mvc" + P, "eps2"], w=["rstdc" + P])
            T.op("act", lambda e: e.activation(out=rstd[p][:], in_=rstd[p][:], func=AF.Exp, scale=-0.5), w=["rstdc" + P])
            yield
            T.op("dve", lambda e: e.tensor_scalar(out=nmr[p][:], in0=mv[p][:, 0:1], scalar1=rstd[p][:, 0:1], scalar2=-1.0, op0=ALU.mult, op1=ALU.mult),
                 r=["mvc" + P, "rstdc" + P], w=["nmrc" + P])
            o_ = of[p]
            on = "of%d" % p
            T.op("act", lambda e: e.activation(out=o_[:], in_=z2[p][:], func=AF.Identity, bias=nmr[p][:, 0:1], scale=rstd[p][:, 0:1]),
                 r=["z2" + P, "rstdc" + P, "nmrc" + P], w=[on])
            yield
            T.op("dve", lambda e: e.tensor_tensor(out=o_[:], in0=o_[:], in1=g_2[:], op=ALU.mult), r=["g_2"], w=[on])
            yield
            T.op("pool", lambda e: e.tensor_tensor(out=o_[:], in0=o_[:], in1=b_2[:], op=ALU.add), r=["b_2"], w=[on])
            T.dma("sp", lambda e: e.dma_start(out=out[(t - 1) * 128:t * 128, :], in_=o_[:]), r=[on], w=["out"])
            yield

        todo = [CT(t) for t in range(1, n_tiles if do_moe else 1)]
        active = []
        rnd = 0
        while todo or active:
            if todo and len(active) < NPC and rnd % 3 == 0:
                active.append(todo.pop(0))
            rnd += 1
            for g in list(active):
                try:
                    next(g)
                except StopIteration:
                    active.remove(g)
    es_cur[0] = es
    T.finish("sp")
    if DEBUG:
        print("sem counts:", {str(k): v for k, v in T.cnt.items() if not isinstance(k, tuple)}, "max dma", max(v for k, v in T.cnt.items() if isinstance(k, tuple)), "nins", T.nins)
    es.close()
    if return_plan:
        return {k: sorted(v) for k, v in T.record.items()}
    return nc


def build_two_pass(**kw):
    plan = build_nc(return_plan=True, **kw)
    return build_nc(plan=plan, **kw)


def _consts():
    c = {}
    c["c_identb"] = np.eye(128, dtype=np.float32).astype(ml_dtypes.bfloat16)
    c["c_identf"] = np.eye(128, dtype=np.float32)
    pos = np.zeros((128, NT), np.float32)
    for t in range(NT):
        for r in range(128):
            pos[r, t] = r if t == 0 else NMETA + 128 * (t - 1) + r
    pos[NMETA:, 0] = 0
    inv = (1.0 / (np.float32(10000.0) ** (np.arange(0, 64, 2, dtype=np.float32) / np.float32(64)))).astype(np.float32)
    ang = (pos[:, :, None] * inv[None, None, :]).astype(np.float32)
    ang = np.concatenate([ang, ang], axis=-1)
    cs_ = np.cos(ang).astype(np.float32)
    sn = np.sin(ang).astype(np.float32)
    sn[:, :, 0:32] *= -1.0
    c["c_cs"] = np.ascontiguousarray(np.concatenate([cs_, sn], axis=-1).transpose(1, 0, 2))
    s = np.arange(128)[:, None]
    tt = np.arange(128)[None, :]
    trim = np.zeros((128, 128), np.float32)
    trim[(s > 63) & (s <= tt)] = 1.0
    trim[(tt < s) & (s <= 63)] = -1.0
    c["c_trim"] = trim
    sel3 = np.zeros((128, 3), np.float32)
    sel3[:64, 0] = 1.0
    sel3[:, 1] = 1.0
    sel3[64:, 2] = 1.0
    c["c_sel3"] = sel3
    c["c_masku"] = np.tile((s <= tt).astype(np.float32), (1, 4))
    v0 = np.zeros((128, 1), np.float32)
    v0[:NMETA] = 1.0
    c["c_valid0"] = v0
    c["c_tris"] = (s < tt).astype(np.float32)
    c["c_ones"] = np.ones((128, 128), np.float32)
    c["c_ebase"] = np.tile((np.arange(NE, dtype=np.float32) * CAP)[None, :], (128, 1))
    c["c_trash"] = (NE * CAP + np.arange(128, dtype=np.float32)).reshape(128, 1)
    c["c_zeros"] = np.zeros((1024, D), dtype=ml_dtypes.bfloat16)
    c["c_piota"] = np.tile(np.arange(NE, dtype=np.float32)[:, None], (1, 128))
    return c


_NC_CACHE = {}


def kernel(**inputs):
    inp = {k: np.asarray(v) for k, v in inputs.items()}
    B = inp["x"].shape[0]
    if "nc" not in _NC_CACHE:
        _NC_CACHE["nc"] = build_two_pass()
    nc = _NC_CACHE["nc"]
    consts = _consts()
    shared = {
        "meta_tokens": inp["meta_tokens"], "ln_emb_g": inp["ln_emb_g"].reshape(1, D), "ln_emb_b": inp["ln_emb_b"].reshape(1, D),
        "w_in": inp["w_in"][0], "lambda_q1": inp["lambda_q1"], "lambda_k1": inp["lambda_k1"],
        "lambda_q2": inp["lambda_q2"], "lambda_k2": inp["lambda_k2"], "subln_g": inp["subln_g"],
        "hgrn_lb_table": inp["hgrn_lb_table"], "hgrn_norm_g": inp["hgrn_norm_g"], "w_out": inp["w_out"][0],
        "ln1_g": inp["ln1_g"], "ln1_b": inp["ln1_b"], "w_router": inp["w_router"][0], "b_router": inp["b_router"],
        "w_gate_up": inp["w_gate_up"][0], "b_gate_up": inp["b_gate_up"][0], "w_down": inp["w_down"][0], "b_down": inp["b_down"][0],
        "ln2_g": inp["ln2_g"], "ln2_b": inp["ln2_b"],
    }
    shared = {k: np.ascontiguousarray(v, dtype=np.float32) for k, v in shared.items()}
    shared.update(consts)
    in_maps = []
    for b in range(B):
        m = dict(shared)
        m["x"] = np.ascontiguousarray(inp["x"][b], dtype=np.float32)
        in_maps.append(m)
    res = run_bass_kernel_spmd(nc, in_maps, core_ids=list(range(B)))
    kernel.last_results = res
    return np.stack([np.asarray(r["out"], dtype=np.float32) for r in res.results], axis=0)
```

```python
import math
import bisect
from contextlib import ExitStack

import numpy as np
import ml_dtypes
import concourse.bass as bass
import concourse.mybir as mybir
from concourse.bass_utils import run_bass_kernel_spmd

F32 = mybir.dt.float32
BF16 = mybir.dt.bfloat16
I32 = mybir.dt.int32
AF = mybir.ActivationFunctionType
ALU = mybir.AluOpType

D = 1024
SEQ = 2048
NT = 17
NMETA = 16
NE = 32
CAP = 448
NS = (CAP + 127) // 128
SROWS = [min(128, CAP - s * 128) for s in range(NS)]
NTRASH = 128
NZ = NE * CAP // 1024
ALPHA = 2.0 ** 0.25
LAM_INIT = 0.2
EPS = 1e-5
DEBUG = False


class Tracker:
    def __init__(self, nc, es, ndma=40, plan=None):
        self.plan = plan
        self.planset = {k: set(v) for k, v in plan.items()} if plan else None
        self.record = {}
        self.nc = nc
        self.E = {"pe": nc.tensor, "act": nc.scalar, "dve": nc.vector, "pool": nc.gpsimd, "sp": nc.sync}
        self.semh = {}
        self.cnt = {}
        for n in self.E:
            self.semh[n] = es.enter_context(nc.semaphore("s_" + n))
            self.cnt[n] = 0
        self.ndma = {"sp": ndma, "pool": 24}
        self.rr = {"sp": 0, "pool": 0}
        for q, n in self.ndma.items():
            for k in range(n):
                self.semh[("d" + q, k)] = es.enter_context(nc.semaphore("s_d%s%d" % (q, k)))
                self.cnt[("d" + q, k)] = 0
        self.waited = {n: {} for n in self.E}
        self.lastw = {}
        self.readers = {}
        self.nins = 0

    def _deps(self, r, w):
        deps = {}

        def add(ev):
            if ev is None:
                return
            k, v = ev
            if deps.get(k, 0) < v:
                deps[k] = v

        for t in r:
            add(self.lastw.get(t))
        for t in w:
            add(self.lastw.get(t))
            for k, v in self.readers.get(t, {}).items():
                add((k, v))
        return deps

    def _wait(self, eng, deps):
        for k, v in deps.items():
            if eng == "pe" and k == "pe":
                continue
            if self.waited[eng].get(k, 0) >= v:
                continue
            if isinstance(k, str):
                self.record.setdefault(k, set()).add(v)
                vv = bisect.bisect_right(self.plan[k], v) if self.plan else v
            else:
                vv = v
            self.E[eng].wait_ge(self.semh[k], vv)
            self.waited[eng][k] = v
            self.nins += 1

    def _commit(self, ev, r, w):
        k, v = ev
        for t in r:
            d = self.readers.setdefault(t, {})
            if d.get(k, 0) < v:
                d[k] = v
        for t in w:
            self.lastw[t] = ev
            self.readers[t] = {}

    def op(self, eng, fn, r=(), w=()):
        self._wait(eng, self._deps(r, w))
        ins = fn(self.E[eng])
        self.cnt[eng] += 1
        if self.planset is None or self.cnt[eng] in self.planset.get(eng, ()):
            ins.then_inc(self.semh[eng], 1)
        self.nins += 1
        self._commit((eng, self.cnt[eng]), r, w)

    def dma(self, q, fn, r=(), w=()):
        k = ("d" + q, self.rr[q])
        self.rr[q] = (self.rr[q] + 1) % self.ndma[q]
        deps = self._deps(r, w)
        if self.cnt[k] > 0 and deps.get(k, 0) < self.cnt[k]:
            deps[k] = self.cnt[k]
        self._wait(q, deps)
        ins = fn(self.E[q])
        self.cnt[k] += 16
        ins.then_inc(self.semh[k], 16)
        self.nins += 1
        self._commit((k, self.cnt[k]), r, w)

    def barrier(self):
        deps = {k: v for k, v in self.cnt.items() if v > 0}
        for eng in self.E:
            self._wait(eng, dict(deps))

    def finish(self, q="sp"):
        deps = {}
        for k in self.cnt:
            if self.cnt[k] > 0:
                deps[k] = self.cnt[k]
        self._wait(q, deps)


def build_nc(n_tiles=NT, do_moe=True, taps=None, tap_tile=1, plan=None, return_plan=False):
    taps = taps or {}
    nc = bass.Bass("TRN2", target_bir_lowering=False)
    es = ExitStack()

    def din(name, shape, dt=F32):
        return nc.dram_tensor(name, list(shape), dt, kind="ExternalInput").ap()

    x = din("x", [SEQ, D])
    meta = din("meta_tokens", [NMETA, D])
    ln_emb_g = din("ln_emb_g", [1, D]); ln_emb_b = din("ln_emb_b", [1, D])
    w_in = din("w_in", [D, 3584])
    lq1 = din("lambda_q1", [1, 64]); lk1 = din("lambda_k1", [1, 64])
    lq2 = din("lambda_q2", [1, 64]); lk2 = din("lambda_k2", [1, 64])
    subln_g = din("subln_g", [1, 128])
    lb_table = din("hgrn_lb_table", [2, 512])
    hg_norm_g = din("hgrn_norm_g", [1, 128])
    w_out = din("w_out", [D, D])
    ln1_g = din("ln1_g", [1, D]); ln1_b = din("ln1_b", [1, D])
    w_router = din("w_router", [D, NE]); b_router = din("b_router", [1, NE])
    w_gu = din("w_gate_up", [NE, D, 2 * D]); b_gu = din("b_gate_up", [NE, 2 * D])
    w_dn = din("w_down", [NE, D, D]); b_dn = din("b_down", [NE, D])
    ln2_g = din("ln2_g", [1, D]); ln2_b = din("ln2_b", [1, D])
    c_identb = din("c_identb", [128, 128], BF16)
    c_identf = din("c_identf", [128, 128])
    c_cs = din("c_cs", [NT, 128, 128])
    c_trim = din("c_trim", [128, 128]); c_sel3 = din("c_sel3", [128, 3])
    c_masku = din("c_masku", [128, 512]); c_valid0 = din("c_valid0", [128, 1])
    c_tris = din("c_tris", [128, 128]); c_ones = din("c_ones", [128, 128])
    c_ebase = din("c_ebase", [128, NE])
    c_piota = din("c_piota", [NE, 128])
    c_trash = din("c_trash", [128, 1])
    c_zeros = din("c_zeros", [1024, D], BF16)

    out = nc.dram_tensor("out", [SEQ, D], F32, kind="ExternalOutput").ap()
    kind_i = "ExternalOutput" if DEBUG else "Internal"
    xs = nc.dram_tensor("xs", [NE * CAP + NTRASH, D], BF16, kind="Internal").ap()
    ys = nc.dram_tensor("ys", [NE * CAP, D], F32, kind="Internal").ap()
    h1d = nc.dram_tensor("h1d", [SEQ, D], F32, kind=kind_i).ap()
    if DEBUG:
        dbg_y = nc.dram_tensor("dbg_y", [NT * 128, D], F32, kind="ExternalOutput").ap()

    T = Tracker(nc, es, plan=plan)
    tap_out = {}
    for nm, (shape, dt) in taps.items():
        tap_out[nm] = nc.dram_tensor("tap_" + nm, list(shape), dt, kind="ExternalOutput").ap()

    def tap(nm, ap, tok, t):
        if nm in tap_out and t == tap_tile:
            T.dma("sp", lambda e: e.dma_start(out=tap_out[nm], in_=ap), r=[tok], w=["tap_" + nm])
    sb_names = [0]

    def sb(shape, dt=F32, name=None):
        sb_names[0] += 1
        return es_cur[0].enter_context(nc.sbuf_tensor(name or "t%d" % sb_names[0], list(shape), dt))

    es_cur = [es]

    PB = [es.enter_context(nc.psum_tensor("pb%d" % i, [128, 512], F32)) for i in range(8)]
    PBn = ["pb%d" % i for i in range(8)]

    def bcast_row(ap_row, n):
        return ap_row.partition_broadcast(128) if False else ap_row.to_broadcast([128, n])

    identb = sb([128, 128], BF16); identf = sb([128, 128])
    valid0 = sb([128, 1])
    ebase = sb([128, NE])
    lam_col = sb([128, 1])
    gates = sb([128, NT, 4]); slots_f = sb([128, NT, 4]); slots_i = sb([128, NT, 4], I32); gslots_i = sb([128, NT, 4], I32)
    trash_col = sb([128, 1])
    bguT = sb([128, 16, NE])
    T.dma("sp", lambda e: e.dma_start(out=identb[:], in_=c_identb), w=["identb"])
    T.dma("sp", lambda e: e.dma_start(out=identf[:], in_=c_identf), w=["identf"])
    T.dma("sp", lambda e: e.dma_start(out=valid0[:], in_=c_valid0), w=["valid0"])
    T.dma("sp", lambda e: e.dma_start(out=ebase[:], in_=c_ebase), w=["ebase"])
    T.dma("sp", lambda e: e.dma_start(out=trash_col[:], in_=c_trash), w=["trash"])

    with ExitStack() as esA:
        es_cur[0] = esA
        w_in_sb = sb([128, 8, 3584], BF16)
        w_out_sb = sb([128, 8, D], BF16)
        w_r_sb = sb([128, 8, NE])
        b_r_b = sb([128, NE])
        kT_all = sb([128, 4, NT * 128], BF16)
        Vaug = sb([128, NT, 4, 130], BF16)
        g_emb = sb([128, D]); b_emb = sb([128, D]); g_1 = sb([128, D]); b_1 = sb([128, D])
        trim = sb([128, 128]); sel3 = sb([128, 3]); masku = sb([128, 512])
        tris = sb([128, 128]); ones = sb([128, 128])
        lb_b = sb([128, 512]); oml_b = sb([128, 512]); ng_b = sb([128, 128]); sg_b = sb([128, 128])
        S_st = sb([128, 4, 128]); cum = sb([128, NE])
        xt = sb([128, D]); hf = sb([128, D]); hT = sb([128, 8, 128], BF16); hf2 = sb([128, D]); hf3 = sb([128, D]); hT2 = sb([128, 8, 128], BF16)
        hb = sb([128, D], BF16); zb = sb([128, D]); cs_t = sb([128, 2, 128])
        st6 = sb([128, 2, 6]); mv = sb([128, 2]); rstd = sb([128, 1]); nmr = sb([128, 1])
        st6b = sb([128, 2, 6]); mvb = sb([128, 2]); rstdb = sb([128, 1]); nmrb = sb([128, 1])
        LNT_A = (st6, mv, rstd, nmr, 'A'); LNT_B = (st6b, mvb, rstdb, nmrb, 'B')
        qr = sb([128, 512], BF16); kr = sb([128, 512], BF16)
        qT = sb([128, 4, 128], BF16)
        PT0 = sb([16, 128], BF16); PTc = [sb([128, 512], BF16), sb([128, 512], BF16)]
        rr_ = sb([128, 2]); t1 = sb([128, 128]); adiff = sb([128, 4, 128]); junk = sb([128, 128]); junk2 = junk
        ss = sb([128, 8]); rs8 = sb([128, 8])
        ybuf = sb([128, D], BF16); yT = sb([128, 8, 128], BF16)
        h_eq = sb([128, 512]); h_qh = sb([128, 512]); h_ef = sb([128, 512]); h_tt = sb([128, 512])
        h_lf = sb([128, 512]); h_kk = sb([128, 512]); h_v = sb([128, 512], BF16)
        h_sgg = sb([128, 512]); h_E = h_eq; tA = h_ef; tB = h_tt
        h_qt = sb([128, 512], BF16); h_kt = sb([128, 512], BF16); qkT = sb([128, 8, 128], BF16)
        dec = sb([128, 12]); Sx = sb([128, 4, 128], BF16); scT = sb([128, 4, 128], BF16)
        o_sb = sb([128, 512]); utmp = sb([128, 4, 128])
        z1 = zb; h1b = sb([128, D], BF16); h1T = zb[:].rearrange("p (a b) -> p a b", b=128)
        lg = sb([128, NE]); top8 = sb([128, 8]); negmx = sb([128, 1]); e4 = sb([128, 4]); den = sb([128, 1])
        maskf = sb([128, NE]); slotf = sb([128, NE]); novf = sb([128, NE]); nk4 = sb([128, 4]); oneh = sb([128, 4, NE]); junk32 = sb([128, NE])

        for blk in (4, 3, 5, 6, 0, 1, 2):
            T.dma("pool", lambda e, blk=blk: e.dma_start(
                out=w_in_sb[:, :, blk * 512:(blk + 1) * 512],
                in_=w_in[:, blk * 512:(blk + 1) * 512].rearrange("(c p) n -> p c n", p=128)), w=["w_in%d" % blk])
        for (dst, src, nm) in ((g_emb, ln_emb_g, "g_emb"), (b_emb, ln_emb_b, "b_emb")):
            T.dma("sp", lambda e, dst=dst, src=src: e.dma_start(out=dst[:], in_=src.to_broadcast([128, D])), w=[nm])
        T.dma("sp", lambda e: e.dma_start(out=cs_t[:, 0, :], in_=c_cs[0]), w=["cs0"])
        T.op("pool", lambda e: e.memset(xt[:], 0.0), w=["xt"])
        T.dma("sp", lambda e: e.dma_start(out=xt[0:NMETA, :], in_=meta), w=["xt"])
        T.dma("sp", lambda e: e.dma_start(out=lb_b[:], in_=lb_table[0:1, :].to_broadcast([128, 512])), w=["lb_b"])
        T.dma("sp", lambda e: e.dma_start(out=oml_b[:], in_=lb_table[1:2, :].to_broadcast([128, 512])), w=["oml_b"])
        for (dst, src, nm) in ((trim, c_trim, "trim"), (sel3, c_sel3, "sel3"), (masku, c_masku, "masku"),
                               (tris, c_tris, "tris"), (ones, c_ones, "ones")):
            T.dma("sp", lambda e, dst=dst, src=src: e.dma_start(out=dst[:], in_=src), w=[nm])
        for (dst, src, nm) in ((g_1, ln1_g, "g_1"), (b_1, ln1_b, "b_1")):
            T.dma("sp", lambda e, dst=dst, src=src: e.dma_start(out=dst[:], in_=src.to_broadcast([128, D])), w=[nm])
        T.dma("sp", lambda e: e.dma_start(out=ng_b[:], in_=hg_norm_g.to_broadcast([128, 128])), w=["ng_b"])
        T.dma("sp", lambda e: e.dma_start(out=sg_b[:], in_=subln_g.to_broadcast([128, 128])), w=["sg_b"])
        T.dma("sp", lambda e: e.dma_start(out=b_r_b[:], in_=b_router.to_broadcast([128, NE])), w=["b_r_b"])
        T.dma("sp", lambda e: e.dma_start(out=w_r_sb[:], in_=w_router.rearrange("(c p) n -> p c n", p=128)), w=["w_r"])
        T.op("dve", lambda e: e.tensor_tensor(out=oml_b[:], in0=oml_b[:], in1=lb_b[:], op=ALU.subtract),
             r=["lb_b"], w=["oml_b"])
        T.op("act", lambda e: e.activation(out=oml_b[:], in_=oml_b[:], func=AF.Exp), w=["oml_b"])
        T.op("dve", lambda e: e.tensor_scalar(out=oml_b[:], in0=oml_b[:], scalar1=1.0, scalar2=None, op0=ALU.add), w=["oml_b"])
        T.op("dve", lambda e: e.reciprocal(out=lb_b[:], in_=oml_b[:]), r=["oml_b"], w=["lb_b"])
        T.op("dve", lambda e: e.tensor_scalar(out=oml_b[:], in0=lb_b[:], scalar1=-1.0, scalar2=1.0, op0=ALU.mult, op1=ALU.add),
             r=["lb_b"], w=["oml_b"])
        tap("lb_b", lb_b[:], "lb_b", tap_tile)
        for blk in range(7):
            tap("w_in%d" % blk, w_in_sb[:, :, blk * 512:(blk + 1) * 512], "w_in%d" % blk, tap_tile)
        tap("oml_b", oml_b[:], "oml_b", tap_tile)
        tap("sg_b", sg_b[:], "sg_b", tap_tile)
        tap("lam", lam_col[:], "lam", tap_tile)
        tap("bguT", bguT[:].rearrange("p a b -> p (a b)"), "bguT", tap_tile)
        T.op("dve", lambda e: e.memset(S_st[:], 0.0), w=["S"])
        T.op("dve", lambda e: e.memset(scT[:], 0.0), w=["scT"])
        T.op("dve", lambda e: e.memset(cum[:], 0.0), w=["cum"])
        T.op("pool", lambda e: e.memset(Vaug[:, :, :, 128:130], 1.0), w=["Vones"])
        T.op("pool", lambda e: e.memset(PTc[0][:], 0.0), w=["PTc0"])
        T.op("pool", lambda e: e.memset(PTc[1][:], 0.0), w=["PTc1"])

        def layer_norm(src_ap, src_tok, g_t, b_t, g_tok, b_tok, dst, dst_tok):
            for hh in range(2):
                T.op("dve", lambda e, hh=hh: e.bn_stats(out=st6[:, hh, :], in_=src_ap[:, hh * 512:(hh + 1) * 512]),
                     r=[src_tok], w=["st6"])
            T.op("dve", lambda e: e.bn_aggr(out=mv[:], in_=st6[:].rearrange("p a b -> p (a b)")), r=["st6"], w=["mv"])
            T.op("act", lambda e: e.activation(out=rstd[:], in_=mv[:, 1:2], func=AF.Ln, bias=eps_col[:], scale=1.0),
                 r=["mv", "eps"], w=["rstd"])
            T.op("act", lambda e: e.activation(out=rstd[:], in_=rstd[:], func=AF.Exp, scale=-0.5), w=["rstd"])
            T.op("dve", lambda e: e.tensor_scalar(out=nmr[:], in0=mv[:, 0:1], scalar1=rstd[:, 0:1], scalar2=-1.0,
                                                  op0=ALU.mult, op1=ALU.mult), r=["mv", "rstd"], w=["nmr"])
            T.op("act", lambda e: e.activation(out=dst[:], in_=src_ap[:], func=AF.Identity, bias=nmr[:, 0:1], scale=rstd[:, 0:1]),
                 r=[src_tok, "rstd", "nmr"], w=[dst_tok])
            T.op("dve", lambda e: e.tensor_tensor(out=dst[:], in0=dst[:], in1=g_t[:], op=ALU.mult), r=[g_tok], w=[dst_tok])
            T.op("dve", lambda e: e.tensor_tensor(out=dst[:], in0=dst[:], in1=b_t[:], op=ALU.add), r=[b_tok], w=[dst_tok])

        eps_col = sb([128, 1]); one_col = sb([128, 1])
        T.op("dve", lambda e: e.memset(eps_col[:], EPS), w=["eps"])
        T.op("dve", lambda e: e.memset(one_col[:], 1.0), w=["one"])

        def transpose8(src, src_tok, dst, dst_tok, n=8, evac="act"):
            pv = PB[2][:].bitcast(BF16)
            for c in range(n):
                T.op("pe", lambda e, c=c: e.transpose(pv[:, c * 128:(c + 1) * 128], src[:, c * 128:(c + 1) * 128], identb[:]),
                     r=[src_tok, "identb"], w=[PBn[2]])
            if evac == "act":
                T.op("act", lambda e: e.activation(out=dst[:].rearrange("p a b -> p (a b)"), in_=pv[:, 0:n * 128], func=AF.Identity),
                     r=[PBn[2]], w=[dst_tok])
            else:
                T.op("dve", lambda e: e.tensor_copy(out=dst[:].rearrange("p a b -> p (a b)"), in_=pv[:, 0:n * 128]),
                     r=[PBn[2]], w=[dst_tok])

        def sigmoid_from_exp(buf, tok):
            T.op("act", lambda e: e.activation(out=buf[:], in_=buf[:], func=AF.Ln, bias=one_col[:], scale=1.0), r=["one"], w=[tok])
            T.op("act", lambda e: e.activation(out=buf[:], in_=buf[:], func=AF.Exp, scale=-1.0), w=[tok])


        hfb = [hf, hf2, hf3]; hTb = [hT, hT2]

        def interleave(gens, est, delay=None):
            n = len(gens)
            delay = list(delay) if delay else [0] * n
            done = [0] * n
            alive = [True] * n
            total = 0
            while any(alive):
                best = None
                for i in range(n):
                    if not alive[i]:
                        continue
                    if delay[i] > total and any(alive[j] and delay[j] <= total for j in range(n)):
                        continue
                    if best is None or done[i] / est[i] < done[best] / est[best]:
                        best = i
                try:
                    next(gens[best])
                    done[best] += 1
                except StopIteration:
                    alive[best] = False
                total += 1

        def F1(t):
            p = t % 2
            if t == 0:
                pass
            else:
                T.dma("sp", lambda e: e.dma_start(out=cs_t[:, p, :], in_=c_cs[t]), w=["cs%d" % p])
                T.dma("sp", lambda e: e.dma_start(out=xt[:], in_=x[(t - 1) * 128:t * 128, :]), w=["xt"])
            yield
            for _ in layer_norm_g(xt, "xt", g_emb, b_emb, "g_emb", "b_emb", hfb[t % 3], "hf%d" % (t % 3), LNT_A):
                yield
            T.op("act", lambda e: e.activation(out=hb[:], in_=hfb[t % 3][:], func=AF.Identity), r=["hf%d" % (t % 3)], w=["hb"])
            yield
            pv = PB[2][:].bitcast(BF16)
            for c in range(8):
                T.op("pe", lambda e, c=c: e.transpose(pv[:, c * 128:(c + 1) * 128], hb[:, c * 128:(c + 1) * 128], identb[:]),
                     r=["hb", "identb"], w=[PBn[2]])
            T.op("act", lambda e: e.activation(out=hTb[p][:].rearrange("p a b -> p (a b)"), in_=pv[:, 0:1024], func=AF.Identity),
                 r=[PBn[2]], w=["hT%d" % p])
            yield

        def layer_norm_g(src_ap, src_tok, g_t, b_t, g_tok, b_tok, dst, dst_tok, tmp):
            st6_, mv_, rstd_, nmr_, sfx = tmp
            for hh in range(2):
                T.op("dve", lambda e, hh=hh: e.bn_stats(out=st6_[:, hh, :], in_=src_ap[:, hh * 512:(hh + 1) * 512]),
                     r=[src_tok], w=["st6" + sfx])
            T.op("dve", lambda e: e.bn_aggr(out=mv_[:], in_=st6_[:].rearrange("p a b -> p (a b)")), r=["st6" + sfx], w=["mv" + sfx])
            yield
            T.op("act", lambda e: e.activation(out=rstd_[:], in_=mv_[:, 1:2], func=AF.Ln, bias=eps_col[:], scale=1.0),
                 r=["mv" + sfx, "eps"], w=["rstd" + sfx])
            T.op("act", lambda e: e.activation(out=rstd_[:], in_=rstd_[:], func=AF.Exp, scale=-0.5), w=["rstd" + sfx])
            yield
            T.op("dve", lambda e: e.tensor_scalar(out=nmr_[:], in0=mv_[:, 0:1], scalar1=rstd_[:, 0:1], scalar2=-1.0,
                                                  op0=ALU.mult, op1=ALU.mult), r=["mv" + sfx, "rstd" + sfx], w=["nmr" + sfx])
            T.op("act", lambda e: e.activation(out=dst[:], in_=src_ap[:], func=AF.Identity, bias=nmr_[:, 0:1], scale=rstd_[:, 0:1]),
                 r=[src_tok, "rstd" + sfx, "nmr" + sfx], w=[dst_tok])
            yield
            T.op("dve", lambda e: e.tensor_tensor(out=dst[:], in0=dst[:], in1=g_t[:], op=ALU.mult), r=[g_tok], w=[dst_tok])
            T.op("pool", lambda e: e.tensor_tensor(out=dst[:], in0=dst[:], in1=b_t[:], op=ALU.add), r=[b_tok], w=[dst_tok])
            yield

        def F2a(t):
            p = t % 2
            hTp = hTb[p]
            hTn = "hT%d" % p
            csn = "cs%d" % p

            pos = [0]

            def inproj(blk):
                bk = pos[0] % 2
                pos[0] += 1
                pb = PB[bk]
                for c in range(8):
                    T.op("pe", lambda e, c=c: e.matmul(pb[:], lhsT=hTp[:, c, :], rhs=w_in_sb[:, c, blk * 512:(blk + 1) * 512],
                                                        start=(c == 0), stop=(c == 7)),
                         r=[hTn, "w_in%d" % blk], w=[PBn[bk]])
                return pb, PBn[bk]

            def rope(pb, pbn, dst, dst_tok):
                pv = pb[:].rearrange("p (a b) -> p a b", b=64)
                pv4 = pb[:].rearrange("p (a h b) -> p a h b", h=2, b=32)
                tB4 = tB[:].rearrange("p (a h b) -> p a h b", h=2, b=32)
                T.op("dve", lambda e: e.tensor_tensor(out=tA[:].rearrange("p (a b) -> p a b", b=64), in0=pv,
                                                      in1=cs_t[:, p:p + 1, 0:64].to_broadcast([128, 8, 64]), op=ALU.mult),
                     r=[pbn, csn], w=["h_ef"])
                T.op("dve", lambda e: e.tensor_tensor(out=tB4[:, :, 0, :], in0=pv4[:, :, 1, :],
                                                      in1=cs_t[:, p:p + 1, 64:96].to_broadcast([128, 8, 32]), op=ALU.mult),
                     r=[pbn, csn], w=["h_tt"])
                T.op("dve", lambda e: e.tensor_tensor(out=tB4[:, :, 1, :], in0=pv4[:, :, 0, :],
                                                      in1=cs_t[:, p:p + 1, 96:128].to_broadcast([128, 8, 32]), op=ALU.mult),
                     r=[pbn, csn], w=["h_tt"])
                T.op("pool", lambda e: e.tensor_tensor(out=dst[:], in0=tA[:], in1=tB[:], op=ALU.add), r=["h_ef", "h_tt"], w=[dst_tok])

            pb, pbn = inproj(0)
            rope(pb, pbn, qr, "qr")
            yield
            pb, pbn = inproj(1)
            rope(pb, pbn, kr, "kr")
            yield
            pv2 = PB[2][:].bitcast(BF16)
            for h in range(4):
                T.op("pe", lambda e, h=h: e.transpose(pv2[:, h * 128:(h + 1) * 128], qr[:, h * 128:(h + 1) * 128], identb[:]),
                     r=["qr", "identb"], w=[PBn[2]])
            for h in range(4):
                T.op("pe", lambda e, h=h: e.transpose(pv2[:, (4 + h) * 128:(5 + h) * 128], kr[:, h * 128:(h + 1) * 128], identb[:]),
                     r=["kr", "identb"], w=[PBn[2]])
            T.op("act", lambda e: e.activation(out=qT[:].rearrange("p a b -> p (a b)"), in_=pv2[:, 0:512], func=AF.Identity),
                 r=[PBn[2]], w=["qT"])
            T.op("act", lambda e: e.activation(out=kT_all[:, :, t * 128:(t + 1) * 128],
                                               in_=pv2[:, 512:1024].rearrange("p (a b) -> p a b", b=128), func=AF.Identity),
                 r=[PBn[2]], w=["kT%d" % t])
            yield
            pb, pbn = inproj(2)
            T.op("act", lambda e: e.activation(out=Vaug[:, t, :, 0:128], in_=pb[:].rearrange("p (a b) -> p a b", b=128), func=AF.Identity),
                 r=[pbn], w=["V%d" % t])
            yield

        def F2b(t):
            p = t % 2
            hTp = hTb[p]
            hTn = "hT%d" % p
            csn = "cs%d" % p

            pos = [0]

            def inproj(blk):
                bk = pos[0] % 2
                pos[0] += 1
                pb = PB[bk]
                for c in range(8):
                    T.op("pe", lambda e, c=c: e.matmul(pb[:], lhsT=hTp[:, c, :], rhs=w_in_sb[:, c, blk * 512:(blk + 1) * 512],
                                                        start=(c == 0), stop=(c == 7)),
                         r=[hTn, "w_in%d" % blk], w=[PBn[bk]])
                return pb, PBn[bk]

            def rope(pb, pbn, dst, dst_tok):
                pv = pb[:].rearrange("p (a b) -> p a b", b=64)
                pv4 = pb[:].rearrange("p (a h b) -> p a h b", h=2, b=32)
                tB4 = tB[:].rearrange("p (a h b) -> p a h b", h=2, b=32)
                T.op("dve", lambda e: e.tensor_tensor(out=tA[:].rearrange("p (a b) -> p a b", b=64), in0=pv,
                                                      in1=cs_t[:, p:p + 1, 0:64].to_broadcast([128, 8, 64]), op=ALU.mult),
                     r=[pbn, csn], w=["h_ef"])
                T.op("dve", lambda e: e.tensor_tensor(out=tB4[:, :, 0, :], in0=pv4[:, :, 1, :],
                                                      in1=cs_t[:, p:p + 1, 64:96].to_broadcast([128, 8, 32]), op=ALU.mult),
                     r=[pbn, csn], w=["h_tt"])
                T.op("dve", lambda e: e.tensor_tensor(out=tB4[:, :, 1, :], in0=pv4[:, :, 0, :],
                                                      in1=cs_t[:, p:p + 1, 96:128].to_broadcast([128, 8, 32]), op=ALU.mult),
                     r=[pbn, csn], w=["h_tt"])
                T.op("pool", lambda e: e.tensor_tensor(out=dst[:], in0=tA[:], in1=tB[:], op=ALU.add), r=["h_ef", "h_tt"], w=[dst_tok])

            def sig_steps(buf, tok):
                T.op("act", lambda e: e.activation(out=buf[:], in_=buf[:], func=AF.Ln, bias=one_col[:], scale=1.0), r=["one"], w=[tok])
                yield
                T.op("act", lambda e: e.activation(out=buf[:], in_=buf[:], func=AF.Exp, scale=-1.0), w=[tok])
                yield

            pb, pbn = inproj(4)
            yield
            T.op("act", lambda e: e.activation(out=h_ef[:], in_=pb[:], func=AF.Exp, scale=-1.0), r=[pbn], w=["h_ef"])
            yield
            yield from sig_steps(h_ef, "h_ef")
            T.op("dve", lambda e: e.tensor_tensor(out=h_tt[:], in0=h_ef[:], in1=oml_b[:], op=ALU.mult), r=["h_ef", "oml_b"], w=["h_tt"])
            yield
            T.op("dve", lambda e: e.tensor_tensor(out=h_ef[:], in0=h_tt[:], in1=lb_b[:], op=ALU.add), r=["h_tt", "lb_b"], w=["h_ef"])
            T.op("pool", lambda e: e.tensor_tensor(out=h_kk[:], in0=oml_b[:], in1=h_tt[:], op=ALU.subtract), r=["h_tt", "oml_b"], w=["h_kk"])
            yield
            T.op("act", lambda e: e.activation(out=h_lf[:], in_=h_ef[:], func=AF.Ln), r=["h_ef"], w=["h_lf"])
            if t == 0:
                T.op("dve", lambda e: e.tensor_scalar(out=h_lf[:], in0=h_lf[:], scalar1=valid0[:, 0:1], scalar2=None, op0=ALU.mult),
                     r=["valid0"], w=["h_lf"])
                T.op("dve", lambda e: e.tensor_scalar(out=h_kk[:], in0=h_kk[:], scalar1=valid0[:, 0:1], scalar2=None, op0=ALU.mult),
                     r=["valid0"], w=["h_kk"])
            yield
            pb, pbn = inproj(3)
            yield
            T.op("act", lambda e: e.activation(out=h_eq[:], in_=pb[:], func=AF.Exp, scale=-1.0), r=[pbn], w=["h_eq"])
            yield
            yield from sig_steps(h_eq, "h_eq")
            T.op("dve", lambda e: e.scalar_tensor_tensor(out=h_qh[:], in0=pb[:], scalar=128.0 ** -0.5, in1=h_eq[:],
                                                         op0=ALU.mult, op1=ALU.mult), r=[pbn, "h_eq"], w=["h_qh"])
            yield
            pb, pbn = inproj(5)
            T.op("act", lambda e: e.activation(out=h_v[:], in_=pb[:], func=AF.Identity), r=[pbn], w=["h_v"])
            yield
            pb, pbn = inproj(6)
            yield
            T.op("act", lambda e: e.activation(out=h_eq[:], in_=pb[:], func=AF.Exp, scale=-1.0), r=[pbn], w=["h_eq"])
            yield
            yield from sig_steps(h_eq, "h_eq")
            T.op("dve", lambda e: e.scalar_tensor_tensor(out=h_sgg[:], in0=pb[:], scalar=1.0, in1=h_eq[:],
                                                         op0=ALU.mult, op1=ALU.mult), r=[pbn, "h_eq"], w=["h_sgg"])
            T.op("pool", lambda e: e.tensor_tensor(out=h_sgg[:].rearrange("p (a b) -> p a b", b=128),
                                                   in0=h_sgg[:].rearrange("p (a b) -> p a b", b=128),
                                                   in1=ng_b[:].rearrange("p (a b) -> p a b", a=1).to_broadcast([128, 4, 128]),
                                                   op=ALU.mult), r=["ng_b"], w=["h_sgg"])
            yield

        def ATT(t):
            nch = t // 4 + 1
            steps = [(h, m, ch) for h in range(4) for m in range(2) for ch in range(nch)]

            def scores(i):
                h, m, ch = steps[i]
                ps = slice(m * 64, (m + 1) * 64)
                spb = PB[3 + (i % 2)]; spn = PBn[3 + (i % 2)]
                ptb = PTc[i % 2]; ptn = "PTc%d" % (i % 2)
                j0 = ch * 4; j1 = min(t, j0 + 3); nj = j1 - j0 + 1
                for jj in range(nj):
                    j = j0 + jj
                    T.op("pe", lambda e, j=j, jj=jj: e.matmul(spb[:, jj * 128:(jj + 1) * 128], lhsT=kT_all[ps, h, j * 128:(j + 1) * 128],
                                                              rhs=qT[ps, h, :], start=True, stop=True), r=["kT%d" % j, "qT"], w=[spn])
                T.op("act", lambda e: e.activation(out=ptb[:, 0:nj * 128], in_=spb[:, 0:nj * 128], func=AF.Exp, scale=0.125), r=[spn], w=[ptn])
                if j1 == t:
                    jj = nj - 1
                    T.op("dve", lambda e: e.memset(ptb[64:128, jj * 128:jj * 128 + 64], 0.0), w=[ptn])

            def pv(i):
                h, m, ch = steps[i]
                acc = PB[5 + (h % 2)]; accn = PBn[5 + (h % 2)]
                accv = acc[:, 0:260].rearrange("p (m c) -> p m c", c=130)
                ptb = PTc[i % 2]; ptn = "PTc%d" % (i % 2)
                j0 = ch * 4; j1 = min(t, j0 + 3); nj = j1 - j0 + 1
                for jj in range(nj):
                    j = j0 + jj
                    if j == 0:
                        T.op("pe", lambda e: e.matmul(accv[:, m, 0:129], lhsT=ptb[0:16, 0:128], rhs=Vaug[0:16, 0, h, 0:129],
                                                      start=True, stop=(t == 0)), r=[ptn, "Vones", "V0"], w=[accn])
                    else:
                        T.op("pe", lambda e, j=j, jj=jj: e.matmul(accv[:, m, 0:129], lhsT=ptb[:, jj * 128:(jj + 1) * 128], rhs=Vaug[:, j, h, 0:129],
                                                                  start=False, stop=(j == t)), r=[ptn, "Vones", "V%d" % j], w=[accn])
                if m == 1 and ch == nch - 1:
                    T.op("dve", lambda e: e.reciprocal(out=rr_[:], in_=accv[:, :, 128]), r=[accn], w=["rr"])
                    T.op("dve", lambda e: e.tensor_scalar(out=rr_[:, 1:2], in0=rr_[:, 1:2], scalar1=lam_col[:, 0:1], scalar2=None, op0=ALU.mult),
                         r=["lam"], w=["rr"])
                    T.op("dve", lambda e: e.tensor_scalar(out=t1[:], in0=accv[:, 1, 0:128], scalar1=rr_[:, 1:2], scalar2=None, op0=ALU.mult),
                         r=[accn, "rr"], w=["t1"])
                    T.op("dve", lambda e: e.scalar_tensor_tensor(out=adiff[:, h, :], in0=accv[:, 0, 0:128], scalar=rr_[:, 0:1],
                                                                 in1=t1[:], op0=ALU.mult, op1=ALU.subtract),
                         r=[accn, "rr", "t1"], w=["adiff"])
                    T.op("dve", lambda e: e.scalar_tensor_tensor(out=junk[:], in0=adiff[:, h, :], scalar=1.0, in1=adiff[:, h, :],
                                                                 op0=ALU.mult, op1=ALU.mult, accum_out=ss[:, h:h + 1]),
                         r=["adiff"], w=["junk", "ss"])

            scores(0)
            for i in range(len(steps)):
                if i + 1 < len(steps):
                    scores(i + 1)
                pv(i)
                yield

        def HG(t):
            T.op("pe", lambda e: e.matmul(PB[3][:], lhsT=trim[:], rhs=h_lf[:], start=True, stop=True), r=["trim", "h_lf"], w=[PBn[3]])
            for h in range(4):
                T.op("pe", lambda e, h=h: e.matmul(PB[4][:, h * 3:(h + 1) * 3], lhsT=h_lf[:, h * 128:(h + 1) * 128], rhs=sel3[:],
                                                   start=True, stop=True), r=["sel3", "h_lf"], w=[PBn[4]])
            yield
            T.op("act", lambda e: e.activation(out=dec[:], in_=PB[4][:, 0:12], func=AF.Exp), r=[PBn[4]], w=["dec"])
            T.op("act", lambda e: e.activation(out=h_E[:], in_=PB[3][:], func=AF.Exp), r=[PBn[3]], w=["h_eq"])
            yield
            T.op("dve", lambda e: e.tensor_tensor(out=h_qt[:], in0=h_qh[:], in1=h_E[:], op=ALU.mult), r=["h_qh", "h_eq"], w=["h_qt"])
            T.op("act", lambda e: e.activation(out=h_E[:], in_=PB[3][:], func=AF.Exp, scale=-1.0), r=[PBn[3]], w=["h_eq"])
            yield
            T.op("dve", lambda e: e.tensor_tensor(out=h_kt[:], in0=h_kk[:], in1=h_E[:], op=ALU.mult), r=["h_kk", "h_eq"], w=["h_kt"])
            yield
            pv2 = PB[2][:].bitcast(BF16)
            for h in range(4):
                T.op("pe", lambda e, h=h: e.transpose(pv2[:, h * 128:(h + 1) * 128], h_qt[:, h * 128:(h + 1) * 128], identb[:]),
                     r=["h_qt", "identb"], w=[PBn[2]])
            for h in range(4):
                T.op("pe", lambda e, h=h: e.transpose(pv2[:, (4 + h) * 128:(5 + h) * 128], h_kt[:, h * 128:(h + 1) * 128], identb[:]),
                     r=["h_kt", "identb"], w=[PBn[2]])
            T.op("act", lambda e: e.activation(out=qkT[:].rearrange("p a b -> p (a b)"), in_=pv2[:, 0:1024], func=AF.Identity),
                 r=[PBn[2]], w=["qkT"])
            yield
            for h in range(4):
                T.op("act", lambda e, h=h: e.activation(out=Sx[:, h, :], in_=S_st[:, h, :], func=AF.Identity, scale=dec[:, 3 * h:3 * h + 1]),
                     r=["S", "dec"], w=["Sx"])
            yield
            for h in range(4):
                T.op("pe", lambda e, h=h: e.matmul(PB[3][0:64, h * 128:(h + 1) * 128], lhsT=qkT[:, 4 + h, 0:64], rhs=qkT[:, h, :], start=True, stop=True),
                     r=["qkT"], w=[PBn[3]])
                T.op("pe", lambda e, h=h: e.matmul(PB[3][64:128, h * 128 + 64:(h + 1) * 128], lhsT=qkT[:, 4 + h, 64:128], rhs=qkT[:, h, 64:128],
                                                   start=True, stop=True), r=["qkT"], w=[PBn[3]])
            mk3 = masku[:].bitcast(I32).rearrange("p (a b) -> p a b", b=128)
            pb3 = PB[3][:].rearrange("p (a b) -> p a b", b=128)
            T.op("dve", lambda e: e.copy_predicated(out=scT[0:64, :, :].rearrange("p a b -> p (a b)"), mask=masku[0:64, :].bitcast(I32), data=PB[3][0:64, :]),
                 r=[PBn[3], "masku"], w=["scT"])
            T.op("dve", lambda e: e.copy_predicated(out=scT[64:128, :, 64:128], mask=mk3[64:128, :, 64:128], data=pb3[64:128, :, 64:128]),
                 r=[PBn[3], "masku"], w=["scT"])
            yield
            for h in range(4):
                T.op("pe", lambda e, h=h: e.matmul(PB[4][:, h * 128:(h + 1) * 128], lhsT=scT[:, h, :], rhs=h_v[:, h * 128:(h + 1) * 128],
                                                   start=True, stop=False), r=["scT", "h_v"], w=[PBn[4]])
                T.op("pe", lambda e, h=h: e.matmul(PB[4][:, h * 128:(h + 1) * 128], lhsT=qkT[:, h, :], rhs=Sx[:, h, :],
                                                   start=False, stop=True), r=["qkT", "Sx"], w=[PBn[4]])
            for h in range(4):
                T.op("pe", lambda e, h=h: e.matmul(PB[3][:, h * 128:(h + 1) * 128], lhsT=h_kt[:, h * 128:(h + 1) * 128], rhs=h_v[:, h * 128:(h + 1) * 128],
                                                   start=True, stop=True), r=["h_kt", "h_v"], w=[PBn[3]])
            yield
            for h in range(4):
                T.op("act", lambda e, h=h: e.activation(out=utmp[:, h, :], in_=PB[3][:, h * 128:(h + 1) * 128], func=AF.Identity,
                                                        scale=dec[:, 3 * h + 2:3 * h + 3]), r=[PBn[3], "dec"], w=["utmp"])
                T.op("dve", lambda e, h=h: e.scalar_tensor_tensor(out=S_st[:, h, :], in0=S_st[:, h, :], scalar=dec[:, 3 * h + 1:3 * h + 2], in1=utmp[:, h, :],
                                                                  op0=ALU.mult, op1=ALU.add), r=["utmp", "dec"], w=["S"])
                if h % 2 == 1:
                    yield
            if t >= 1:
                T.op("act", lambda e: e.activation(out=o_sb[:], in_=PB[4][:], func=AF.Identity), r=[PBn[4]], w=["o_sb"])
                yield
                for h in range(4):
                    T.op("dve", lambda e, h=h: e.scalar_tensor_tensor(out=junk2[:], in0=o_sb[:, h * 128:(h + 1) * 128], scalar=1.0,
                                                                      in1=o_sb[:, h * 128:(h + 1) * 128], op0=ALU.mult, op1=ALU.mult,
                                                                      accum_out=ss[:, 4 + h:5 + h]), r=["o_sb"], w=["junk", "ss"])
                yield
                T.op("pool", lambda e: e.tensor_tensor(out=o_sb[:], in0=o_sb[:], in1=h_sgg[:], op=ALU.mult), r=["h_sgg"], w=["o_sb"])
                yield

        def YB(t):
            p = t % 3
            hfp = hfb[p]; hfn = "hf%d" % p
            T.op("act", lambda e: e.activation(out=rs8[:], in_=ss[:], func=AF.Ln, bias=eps_col[:], scale=1.0 / 128.0), r=["ss", "eps"], w=["rs8"])
            T.op("act", lambda e: e.activation(out=rs8[:], in_=rs8[:], func=AF.Exp, scale=-0.5), w=["rs8"])
            for h in range(4):
                T.op("dve", lambda e, h=h: e.scalar_tensor_tensor(out=ybuf[:, h * 128:(h + 1) * 128], in0=adiff[:, h, :], scalar=rs8[:, h:h + 1],
                                                                  in1=sg_b[:], op0=ALU.mult, op1=ALU.mult), r=["adiff", "rs8", "sg_b"], w=["ybuf"])
                T.op("act", lambda e, h=h: e.activation(out=ybuf[:, 512 + h * 128:512 + (h + 1) * 128], in_=o_sb[:, h * 128:(h + 1) * 128],
                                                        func=AF.Identity, scale=rs8[:, 4 + h:5 + h]), r=["o_sb", "rs8"], w=["ybuf"])
            if DEBUG:
                T.op("pool", lambda e: e.tensor_copy(out=z1[:], in_=ybuf[:]), r=["ybuf"], w=["zb"])
                T.dma("sp", lambda e: e.dma_start(out=dbg_y[t * 128:(t + 1) * 128, :], in_=z1[:]), r=["zb"], w=["dbg_y"])
            yield
            pv = PB[2][:].bitcast(BF16)
            for c in range(8):
                T.op("pe", lambda e, c=c: e.transpose(pv[:, c * 128:(c + 1) * 128], ybuf[:, c * 128:(c + 1) * 128], identb[:]),
                     r=["ybuf", "identb"], w=[PBn[2]])
            T.op("act", lambda e: e.activation(out=yT[:].rearrange("p a b -> p (a b)"), in_=pv[:, 0:1024], func=AF.Identity),
                 r=[PBn[2]], w=["yT"])
            yield
            for hh in range(2):
                for c in range(8):
                    T.op("pe", lambda e, hh=hh, c=c: e.matmul(PB[5 + hh][:], lhsT=yT[:, c, :], rhs=w_out_sb[:, c, hh * 512:(hh + 1) * 512],
                                                              start=(c == 0), stop=(c == 7)), r=["yT", "w_out"], w=[PBn[5 + hh]])
                T.op("dve", lambda e, hh=hh: e.scalar_tensor_tensor(out=z1[:, hh * 512:(hh + 1) * 512], in0=hfp[:, hh * 512:(hh + 1) * 512],
                                                                    scalar=ALPHA, in1=PB[5 + hh][:], op0=ALU.mult, op1=ALU.add),
                     r=[hfn, PBn[5 + hh]], w=["zb"])
                yield
            h1 = hfp
            for _ in layer_norm_g(z1, "zb", g_1, b_1, "g_1", "b_1", h1, hfn, LNT_B):
                yield
            T.dma("sp", lambda e: e.dma_start(out=h1d[(t - 1) * 128:t * 128, :], in_=h1[:]), r=[hfn], w=["h1d"])
            T.op("act", lambda e: e.activation(out=h1b[:], in_=h1[:], func=AF.Identity), r=[hfn], w=["h1b"])
            yield
            for hh in range(2):
                for c4 in range(4):
                    c = hh * 4 + c4
                    T.op("pe", lambda e, hh=hh, c4=c4, c=c: e.transpose(PB[5 + hh][:, c4 * 128:(c4 + 1) * 128], h1[:, c * 128:(c + 1) * 128], identf[:]),
                         r=[hfn, "identf"], w=[PBn[5 + hh]])
                T.op("act", lambda e, hh=hh: e.activation(out=h1T[:, hh * 4:(hh + 1) * 4, :].rearrange("p a b -> p (a b)"), in_=PB[5 + hh][:], func=AF.Identity),
                     r=[PBn[5 + hh]], w=["zb"])
                yield
            for c in range(8):
                T.op("pe", lambda e, c=c: e.matmul(PB[7][:, 0:NE], lhsT=h1T[:, c, :], rhs=w_r_sb[:, c, :], start=(c == 0), stop=(c == 7)),
                     r=["zb", "w_r"], w=[PBn[7]])
            T.op("dve", lambda e: e.tensor_tensor(out=lg[:], in0=PB[7][:, 0:NE], in1=b_r_b[:], op=ALU.add), r=[PBn[7], "b_r_b"], w=["lg"])
            T.op("dve", lambda e: e.max(out=top8[:], in_=lg[:]), r=["lg"], w=["top8"])
            yield
            T.op("dve", lambda e: e.tensor_scalar(out=negmx[:], in0=top8[:, 0:1], scalar1=-1.0, scalar2=None, op0=ALU.mult), r=["top8"], w=["negmx"])
            T.op("act", lambda e: e.activation(out=e4[:], in_=top8[:, 0:4], func=AF.Exp, bias=negmx[:, 0:1], scale=1.0, accum_out=den[:]),
                 r=["top8", "negmx"], w=["e4", "den"])
            T.op("dve", lambda e: e.tensor_scalar(out=maskf[:], in0=lg[:], scalar1=top8[:, 3:4], scalar2=None, op0=ALU.is_ge),
                 r=["lg", "top8"], w=["maskf"])
            yield
            T.op("pe", lambda e: e.matmul(PB[7][:, 64:64 + NE], lhsT=tris[:], rhs=maskf[:], start=True, stop=False), r=["tris", "maskf"], w=[PBn[7]])
            T.op("pe", lambda e: e.matmul(PB[7][:, 64:64 + NE], lhsT=ones[:], rhs=cum[:], start=False, stop=True), r=["ones", "cum"], w=[PBn[7]])
            T.op("dve", lambda e: e.reciprocal(out=den[:], in_=den[:]), r=["den"], w=["den"])
            T.op("dve", lambda e: e.tensor_scalar(out=gates[:, t, :], in0=e4[:], scalar1=den[:, 0:1], scalar2=None, op0=ALU.mult),
                 r=["e4", "den"], w=["gates"])
            yield
            T.op("dve", lambda e: e.tensor_scalar(out=novf[:], in0=PB[7][:, 64:64 + NE], scalar1=float(CAP), scalar2=None, op0=ALU.is_lt),
                 r=[PBn[7]], w=["novf"])
            T.op("dve", lambda e: e.tensor_tensor(out=slotf[:], in0=PB[7][:, 64:64 + NE], in1=ebase[:], op=ALU.add),
                 r=[PBn[7], "ebase"], w=["slotf"])
            T.op("dve", lambda e: e.tensor_tensor(out=cum[:], in0=cum[:], in1=maskf[:], op=ALU.add), r=["maskf"], w=["cum"])
            yield
            T.op("dve", lambda e: e.scalar_tensor_tensor(out=slotf[:], in0=slotf[:], scalar=trash_col[:, 0:1], in1=novf[:], op0=ALU.subtract, op1=ALU.mult),
                 r=["novf", "trash"], w=["slotf"])
            T.op("dve", lambda e: e.tensor_scalar(out=slotf[:], in0=slotf[:], scalar1=trash_col[:, 0:1], scalar2=None, op0=ALU.add), r=["trash"], w=["slotf"])
            yield
            for k in range(4):
                T.op("dve", lambda e, k=k: e.tensor_scalar(out=oneh[:, k, :], in0=lg[:], scalar1=top8[:, k:k + 1], scalar2=None, op0=ALU.is_equal),
                     r=["lg", "top8"], w=["oneh"])
                T.op("dve", lambda e, k=k: e.scalar_tensor_tensor(out=junk32[:], in0=oneh[:, k, :], scalar=1.0, in1=slotf[:], op0=ALU.mult, op1=ALU.mult,
                                                                  accum_out=slots_f[:, t, k:k + 1]), r=["oneh", "slotf"], w=["junk32", "slots_f"])
                T.op("dve", lambda e, k=k: e.scalar_tensor_tensor(out=junk32[:], in0=oneh[:, k, :], scalar=1.0, in1=novf[:], op0=ALU.mult, op1=ALU.mult,
                                                                  accum_out=nk4[:, k:k + 1]), r=["oneh", "novf"], w=["junk32", "nk4"])
                if k % 2 == 1:
                    yield
            T.op("dve", lambda e: e.tensor_scalar(out=slots_f[:, t, :], in0=slots_f[:, t, :], scalar1=0.0, scalar2=float(NE * CAP + NTRASH - 1),
                                                  op0=ALU.max, op1=ALU.min), w=["slots_f"])
            T.op("dve", lambda e: e.tensor_copy(out=slots_i[:, t, :], in_=slots_f[:, t, :]), r=["slots_f"], w=["slots_i"])
            T.op("dve", lambda e: e.tensor_tensor(out=gates[:, t, :], in0=gates[:, t, :], in1=nk4[:], op=ALU.mult), r=["nk4"], w=["gates"])
            T.op("dve", lambda e: e.tensor_tensor(out=slots_f[:, t, :], in0=slots_f[:, t, :], in1=nk4[:], op=ALU.mult), r=["nk4"], w=["slots_f"])
            T.op("dve", lambda e: e.tensor_copy(out=gslots_i[:, t, :], in_=slots_f[:, t, :]), r=["slots_f"], w=["gslots_i"])
            for k in range(4 if do_moe else 0):
                T.dma("pool", lambda e, k=k: e.indirect_dma_start(
                    out=xs, out_offset=bass.IndirectOffsetOnAxis(ap=slots_i[:, t, k:k + 1], axis=0),
                    in_=h1b[:, :], in_offset=None), r=["h1b", "slots_i"] + ["xs_z%d" % i for i in range(NZ)], w=["xs_sc_%d_%d" % (t, k)])
            yield

        def run(g):
            for _ in g:
                pass

        if DEBUG:
            print("phase A sbuf bytes remaining:", nc.sbuf_bytes_remaining)
        run(F1(0))
        run(F2b(0))
        lt = sb([128, 4, 64]); ltmp = sb([128, 64]); lsum = sb([128, 2])
        for i, a_ in enumerate((lq1, lk1, lq2, lk2)):
            T.dma("sp", lambda e, i=i, a_=a_: e.dma_start(out=lt[:, i, :], in_=a_.to_broadcast([128, 64])), w=["lt%d" % i])
        for i in range(2):
            T.op("dve", lambda e, i=i: e.scalar_tensor_tensor(
                out=ltmp[:], in0=lt[:, 2 * i, :], scalar=1.0, in1=lt[:, 2 * i + 1, :],
                op0=ALU.mult, op1=ALU.mult, accum_out=lsum[:, i:i + 1]), r=["lt%d" % (2 * i), "lt%d" % (2 * i + 1)], w=["ltmp", "lsum"])
        T.op("act", lambda e: e.activation(out=lsum[:], in_=lsum[:], func=AF.Exp), r=["lsum"], w=["lsum"])
        T.op("dve", lambda e: e.tensor_scalar(out=lam_col[:], in0=lsum[:, 0:1], scalar1=lsum[:, 1:2],
                                              scalar2=LAM_INIT, op0=ALU.subtract, op1=ALU.add),
             r=["lsum"], w=["lam"])
        T.op("dve", lambda e: e.tensor_scalar(out=sg_b[:], in0=sg_b[:], scalar1=1.0 - LAM_INIT, scalar2=None, op0=ALU.mult), w=["sg_b"])
        for t in range(-1, n_tiles):
            if t >= 0:
                gens = []; est = []; dl = []
                if t >= 1:
                    gens.append(ATT(t)); est.append(8.0 * (t // 4 + 1)); dl.append(0)
                if t + 1 < n_tiles:
                    gens.append(F2b(t + 1)); est.append(20.0); dl.append(1)
                if gens:
                    interleave(gens, est, dl)
            gens = []; est = []; dl = []
            if t >= 1:
                yb = YB(t)
                next(yb)
            if t + 1 < n_tiles:
                gens.append(F2a(t + 1)); est.append(4.0); dl.append(0)
                gens.append(HG(t + 1)); est.append(15.0); dl.append(0)
            if t >= 1:
                gens.append(yb); est.append(18.0); dl.append(1)
            if t + 2 < n_tiles:
                gens.append(F1(t + 2)); est.append(8.0); dl.append(2)
            if gens:
                interleave(gens, est, dl)
            if t == -1:
                for hh in range(2):
                    T.dma("pool", lambda e, hh=hh: e.dma_start(
                        out=w_out_sb[:, :, hh * 512:(hh + 1) * 512],
                        in_=w_out[:, hh * 512:(hh + 1) * 512].rearrange("(c p) n -> p c n", p=128)), w=["w_out"])
                for i in range(NZ):
                    T.dma("pool", lambda e, i=i: e.dma_start(out=xs[i * 1024:(i + 1) * 1024, :], in_=c_zeros), w=["xs_z%d" % i])
    es_cur[0] = es
    T.barrier()

    with ExitStack() as esB:
        es_cur[0] = esB
        if not do_moe:
            NE_run = 0
        else:
            NE_run = NE
        wgu = [sb([128, 8, 2 * D], BF16), sb([128, 8, 2 * D], BF16)]
        wdn = [sb([128, 8, D], BF16), sb([128, 8, D], BF16)]
        bdn_bf = sb([NE, D], BF16); piota = sb([NE, 128]); selE = [sb([NE, 128], BF16), sb([NE, 128], BF16)]
        bguS = sb([128, 8, NE])
        xrows = [sb([128, NS, D], BF16), sb([128, NS, D], BF16)]
        xT = [sb([128, 8, CAP], BF16), sb([128, 8, CAP], BF16)]
        actT = [sb([128, 8, CAP], BF16), sb([128, 8, CAP], BF16)]
        g1 = [sb([128, CAP]), sb([128, CAP])]; u0 = [sb([128, CAP]), sb([128, CAP])]; gs = [sb([128, CAP]), sb([128, CAP])]
        ysb = [sb([128, D]), sb([128, D])]
        SI = 1.0 / 1.702
        bgu_sb = sb([NE, 2 * D])
        T.dma("sp", lambda e: e.dma_start(out=bgu_sb[:], in_=b_gu), w=["bgu_sb"])
        for j2 in range(16):
            j, two = j2 // 2, j2 % 2
            T.op("pe", lambda e, j=j, two=two, j2=j2: e.transpose(
                PB[0][:, j2 * NE:(j2 + 1) * NE], bgu_sb[:, j * 256 + two:(j + 1) * 256:2], identf[0:NE, 0:NE]),
                r=["bgu_sb", "identf"], w=[PBn[0]])
        T.op("dve", lambda e: e.tensor_copy(out=bguT[:].rearrange("p a b -> p (a b)"), in_=PB[0][:, 0:16 * NE]),
             r=[PBn[0]], w=["bguT"])
        T.dma("pool", lambda e: e.dma_start(out=bdn_bf[:], in_=b_dn), w=["bdn_bf"])
        T.dma("sp", lambda e: e.dma_start(out=piota[:], in_=c_piota), w=["piota"])
        T.op("dve", lambda e: e.tensor_scalar(out=bguS[:], in0=bguT[:, 1:16:2, :], scalar1=SI, scalar2=None, op0=ALU.mult), r=["bguT"], w=["bguS"])

        def load_gu(e_):
            b = e_ % 2
            for hh in range(4):
                T.dma("pool", lambda e, hh=hh: e.dma_start(
                    out=wgu[b][:, :, hh * 512:(hh + 1) * 512],
                    in_=w_gu[e_, :, hh * 512:(hh + 1) * 512].rearrange("(c p) n -> p c n", p=128)), w=["wgu%d_%d" % (b, hh)])

        def load_dn(e_):
            b = e_ % 2
            for hh in range(2):
                T.dma("pool", lambda e, hh=hh: e.dma_start(
                    out=wdn[b][:, :, hh * 512:(hh + 1) * 512],
                    in_=w_dn[e_, :, hh * 512:(hh + 1) * 512].rearrange("(c p) n -> p c n", p=128)), w=["wdn%d_%d" % (b, hh)])

        SC_TOK = ["xs_sc_%d_%d" % (t_, k_) for t_ in range(1, n_tiles) for k_ in range(4)] + ["xs_z%d" % i for i in range(NZ)]

        def load_x(e_):
            b = e_ % 2
            nfull = CAP // 128
            T.dma("sp", lambda e: e.dma_start(out=xrows[b][:, 0:nfull, :], in_=xs[e_ * CAP:e_ * CAP + nfull * 128, :].rearrange("(s p) d -> p s d", p=128)),
                  r=SC_TOK, w=["xrows%d" % b])
            if NS > nfull:
                T.dma("sp", lambda e: e.dma_start(out=xrows[b][0:SROWS[-1], nfull, :], in_=xs[e_ * CAP + nfull * 128:(e_ + 1) * CAP, :]),
                      r=SC_TOK, w=["xrows%d_t" % b])

        if do_moe:
            load_gu(0); load_dn(0); load_gu(1); load_dn(1)
            load_x(0)
        for ex in range(NE_run):
            b = ex % 2
            if ex + 1 < NE:
                load_x(ex + 1)
            T.op("dve", lambda e, ex=ex: e.tensor_scalar(out=selE[b][:], in0=piota[:], scalar1=float(ex), scalar2=None, op0=ALU.is_equal),
                 r=["piota"], w=["selE%d" % b])
            for c2 in range(4):
                pbt = PB[2 + (c2 % 2) * 5]
                pbtn = PBn[2 + (c2 % 2) * 5]
                pvb = pbt[:].bitcast(BF16)
                for cc in range(2):
                    c = c2 * 2 + cc
                    for s in range(NS):
                        T.op("pe", lambda e, c=c, cc=cc, s=s, pvb=pvb: e.transpose(
                            pvb[:, cc * CAP + s * 128:cc * CAP + s * 128 + SROWS[s]], xrows[b][0:SROWS[s], s, c * 128:(c + 1) * 128], identb[0:SROWS[s], 0:SROWS[s]]),
                            r=["xrows%d" % b, "xrows%d_t" % b, "identb"], w=[pbtn])
                T.op("act" if c2 % 2 == 0 else "dve",
                     (lambda e, c2=c2, pvb=pvb: e.activation(out=xT[b][:, c2 * 2:c2 * 2 + 2, :].rearrange("p a b -> p (a b)"), in_=pvb[:, 0:2 * CAP], func=AF.Identity))
                     if c2 % 2 == 0 else
                     (lambda e, c2=c2, pvb=pvb: e.tensor_copy(out=xT[b][:, c2 * 2:c2 * 2 + 2, :].rearrange("p a b -> p (a b)"), in_=pvb[:, 0:2 * CAP])),
                     r=[pbtn], w=["xT%d" % b])
            for j in range(8):
                jb = j % 2
                pg = PB[jb * 3]
                pgn = PBn[jb * 3]
                pu = PB[jb * 3 + 1]
                pun = PBn[jb * 3 + 1]
                wtok = "wgu%d_%d" % (b, j // 2)
                for c in range(8):
                    T.op("pe", lambda e, c=c, j=j, pg=pg: e.matmul(pg[:, 0:CAP], lhsT=wgu[b][:, c, j * 256:(j + 1) * 256:2], rhs=xT[b][:, c, :],
                                                                     start=(c == 0), stop=(c == 7)), r=[wtok, "xT%d" % b], w=[pgn])
                for c in range(8):
                    T.op("pe", lambda e, c=c, j=j, pu=pu: e.matmul(pu[:, 0:CAP], lhsT=wgu[b][:, c, j * 256 + 1:(j + 1) * 256:2], rhs=xT[b][:, c, :],
                                                                     start=(c == 0), stop=(c == 7)), r=[wtok, "xT%d" % b], w=[pun])
                T.op("dve", lambda e, j=j, pg=pg, ex=ex, jb=jb: e.tensor_scalar(out=g1[jb][:], in0=pg[:, 0:CAP], scalar1=bguT[:, 2 * j, ex:ex + 1], scalar2=7.0,
                                                                                op0=ALU.add, op1=ALU.min), r=[pgn, "bguT"], w=["g1_%d" % jb])
                T.op("act", lambda e, j=j, pu=pu, ex=ex, jb=jb: e.activation(out=u0[jb][:], in_=pu[:, 0:CAP], func=AF.Identity, bias=bguS[:, j, ex:ex + 1], scale=SI),
                     r=[pun, "bguS"], w=["u0_%d" % jb])
                T.op("act", lambda e, jb=jb: e.activation(out=gs[jb][:], in_=g1[jb][:], func=AF.Silu, scale=1.702), r=["g1_%d" % jb], w=["gs_%d" % jb])
                T.op("dve", lambda e, jb=jb: e.tensor_scalar(out=u0[jb][:], in0=u0[jb][:], scalar1=7.0 * SI, scalar2=-7.0 * SI, op0=ALU.min, op1=ALU.max), w=["u0_%d" % jb])
                T.op("dve", lambda e, j=j, jb=jb: e.scalar_tensor_tensor(out=actT[b][:, j, :], in0=u0[jb][:], scalar=SI, in1=gs[jb][:], op0=ALU.add, op1=ALU.mult),
                     r=["u0_%d" % jb, "gs_%d" % jb], w=["actT%d" % b])
            if ex + 2 < NE:
                load_gu(ex + 2)
            for s in range(NS):
                yb_ = ysb[s % 2]
                ybn = "ysb%d" % (s % 2)
                R = SROWS[s]
                for hh in range(2):
                    py = PB[5 + hh]
                    T.op("pe", lambda e, hh=hh, py=py: e.matmul(py[0:R, :], lhsT=selE[b][:, 0:R], rhs=bdn_bf[:, hh * 512:(hh + 1) * 512], start=True, stop=False),
                         r=["selE%d" % b, "bdn_bf"], w=[PBn[5 + hh]])
                    for j in range(8):
                        T.op("pe", lambda e, j=j, s=s, hh=hh, py=py: e.matmul(py[0:R, :], lhsT=actT[b][:, j, s * 128:s * 128 + R],
                                                                               rhs=wdn[b][:, j, hh * 512:(hh + 1) * 512], start=False, stop=(j == 7)),
                             r=["actT%d" % b, "wdn%d_%d" % (b, hh)], w=[PBn[5 + hh]])
                    T.op("act" if hh == 0 else "dve",
                         (lambda e, hh=hh, py=py, yb_=yb_: e.activation(out=yb_[0:R, hh * 512:(hh + 1) * 512], in_=py[0:R, :], func=AF.Identity)) if hh == 0 else
                         (lambda e, hh=hh, py=py, yb_=yb_: e.tensor_copy(out=yb_[0:R, hh * 512:(hh + 1) * 512], in_=py[0:R, :])),
                         r=[PBn[5 + hh]], w=[ybn])
                T.dma("sp", lambda e, ex=ex, s=s, yb_=yb_: e.dma_start(out=ys[ex * CAP + s * 128:ex * CAP + s * 128 + R, :], in_=yb_[0:R, :]),
                      r=[ybn], w=["ys"])
            if ex + 2 < NE:
                load_dn(ex + 2)
    es_cur[0] = es
    T.barrier()

    with ExitStack() as esC:
        es_cur[0] = esC
        g_2 = sb([128, D]); b_2 = sb([128, D])
        T.dma("sp", lambda e: e.dma_start(out=g_2[:], in_=ln2_g.to_broadcast([128, D])), w=["g_2"])
        T.dma("sp", lambda e: e.dma_start(out=b_2[:], in_=ln2_b.to_broadcast([128, D])), w=["b_2"])
        NPC = 4
        yk = [[sb([128, D]) for _ in range(4)] for _ in range(NPC)]
        h1r = [sb([128, D]) for _ in range(NPC)]
        accf = [sb([128, D]) for _ in range(NPC)]; z2 = [sb([128, D]) for _ in range(NPC)]; of = [sb([128, D]) for _ in range(NPC)]
        st6 = [sb([128, 2, 6]) for _ in range(NPC)]; mv = [sb([128, 2]) for _ in range(NPC)]
        rstd = [sb([128, 1]) for _ in range(NPC)]; nmr = [sb([128, 1]) for _ in range(NPC)]; eps_col = sb([128, 1])
        T.op("dve", lambda e: e.memset(eps_col[:], EPS), w=["eps2"])

        def CT(t):
            p = t % NPC
            P = "_%d" % p
            for k in range(4):
                T.dma("pool", lambda e, k=k: e.indirect_dma_start(
                    out=yk[p][k][:, :], out_offset=None, in_=ys,
                    in_offset=bass.IndirectOffsetOnAxis(ap=gslots_i[:, t, k:k + 1], axis=0)),
                    r=["ys", "gslots_i"], w=["yk%d_%d" % (p, k)])
            T.dma("sp", lambda e: e.dma_start(out=h1r[p][:], in_=h1d[(t - 1) * 128:t * 128, :]), r=["h1d"], w=["h1r" + P])
            yield
            T.op("dve", lambda e: e.tensor_scalar(out=accf[p][:], in0=yk[p][0][:], scalar1=gates[:, t, 0:1], scalar2=None, op0=ALU.mult),
                 r=["yk%d_0" % p, "gates"], w=["accf" + P])
            yield
            for k in range(1, 4):
                T.op("dve", lambda e, k=k: e.scalar_tensor_tensor(out=accf[p][:], in0=yk[p][k][:], scalar=gates[:, t, k:k + 1], in1=accf[p][:],
                                                                  op0=ALU.mult, op1=ALU.add), r=["yk%d_%d" % (p, k), "gates"], w=["accf" + P])
                yield
            T.op("dve", lambda e: e.scalar_tensor_tensor(out=z2[p][:], in0=h1r[p][:], scalar=ALPHA, in1=accf[p][:], op0=ALU.mult, op1=ALU.add),
                 r=["h1r" + P, "accf" + P], w=["z2" + P])
            yield
            for hh in range(2):
                T.op("dve", lambda e, hh=hh: e.bn_stats(out=st6[p][:, hh, :], in_=z2[p][:, hh * 512:(hh + 1) * 512]), r=["z2" + P], w=["st6c" + P])
            T.op("dve", lambda e: e.bn_aggr(out=mv[p][:], in_=st6[p][:].rearrange("p a b -> p (a b)")), r=["st6c" + P], w=["mvc" + P])
            yield
            T.op("act", lambda e: e.activation(out=rstd[p][:], in_=mv[p][:, 1:2], func=AF.Ln, bias=eps_col[:], scale=1.0), r=["mvc" + P, "eps2"], w=["rstdc" + P])
            T.op("act", lambda e: e.activation(out=rstd[p][:], in_=rstd[p][:], func=AF.Exp, scale=-0.5), w=["rstdc" + P])
            yield
            T.op("dve", lambda e: e.tensor_scalar(out=nmr[p][:], in0=mv[p][:, 0:1], scalar1=rstd[p][:, 0:1], scalar2=-1.0, op0=ALU.mult, op1=ALU.mult),
                 r=["mvc" + P, "rstdc" + P], w=["nmrc" + P])
            o_ = of[p]
            on = "of%d" % p
            T.op("act", lambda e: e.activation(out=o_[:], in_=z2[p][:], func=AF.Identity, bias=nmr[p][:, 0:1], scale=rstd[p][:, 0:1]),
                 r=["z2" + P, "rstdc" + P, "nmrc" + P], w=[on])
            yield
            T.op("dve", lambda e: e.tensor_tensor(out=o_[:], in0=o_[:], in1=g_2[:], op=ALU.mult), r=["g_2"], w=[on])
            yield
            T.op("pool", lambda e: e.tensor_tensor(out=o_[:], in0=o_[:], in1=b_2[:], op=ALU.add), r=["b_2"], w=[on])
            T.dma("sp", lambda e: e.dma_start(out=out[(t - 1) * 128:t * 128, :], in_=o_[:]), r=[on], w=["out"])
            yield

        todo = [CT(t) for t in range(1, n_tiles if do_moe else 1)]
        active = []
        rnd = 0
        while todo or active:
            if todo and len(active) < NPC and rnd % 3 == 0:
                active.append(todo.pop(0))
            rnd += 1
            for g in list(active):
                try:
                    next(g)
                except StopIteration:
                    active.remove(g)
    es_cur[0] = es
    T.finish("sp")
    if DEBUG:
        print("sem counts:", {str(k): v for k, v in T.cnt.items() if not isinstance(k, tuple)}, "max dma", max(v for k, v in T.cnt.items() if isinstance(k, tuple)), "nins", T.nins)
    es.close()
    if return_plan:
        return {k: sorted(v) for k, v in T.record.items()}
    return nc


def build_two_pass(**kw):
    plan = build_nc(return_plan=True, **kw)
    return build_nc(plan=plan, **kw)


def _consts():
    c = {}
    c["c_identb"] = np.eye(128, dtype=np.float32).astype(ml_dtypes.bfloat16)
    c["c_identf"] = np.eye(128, dtype=np.float32)
    pos = np.zeros((128, NT), np.float32)
    for t in range(NT):
        for r in range(128):
            pos[r, t] = r if t == 0 else NMETA + 128 * (t - 1) + r
    pos[NMETA:, 0] = 0
    inv = (1.0 / (np.float32(10000.0) ** (np.arange(0, 64, 2, dtype=np.float32) / np.float32(64)))).astype(np.float32)
    ang = (pos[:, :, None] * inv[None, None, :]).astype(np.float32)
    ang = np.concatenate([ang, ang], axis=-1)
    cs_ = np.cos(ang).astype(np.float32)
    sn = np.sin(ang).astype(np.float32)
    sn[:, :, 0:32] *= -1.0
    c["c_cs"] = np.ascontiguousarray(np.concatenate([cs_, sn], axis=-1).transpose(1, 0, 2))
    s = np.arange(128)[:, None]
    tt = np.arange(128)[None, :]
    trim = np.zeros((128, 128), np.float32)
    trim[(s > 63) & (s <= tt)] = 1.0
    trim[(tt < s) & (s <= 63)] = -1.0
    c["c_trim"] = trim
    sel3 = np.zeros((128, 3), np.float32)
    sel3[:64, 0] = 1.0
    sel3[:, 1] = 1.0
    sel3[64:, 2] = 1.0
    c["c_sel3"] = sel3
    c["c_masku"] = np.tile((s <= tt).astype(np.float32), (1, 4))
    v0 = np.zeros((128, 1), np.float32)
    v0[:NMETA] = 1.0
    c["c_valid0"] = v0
    c["c_tris"] = (s < tt).astype(np.float32)
    c["c_ones"] = np.ones((128, 128), np.float32)
    c["c_ebase"] = np.tile((np.arange(NE, dtype=np.float32) * CAP)[None, :], (128, 1))
    c["c_trash"] = (NE * CAP + np.arange(128, dtype=np.float32)).reshape(128, 1)
    c["c_zeros"] = np.zeros((1024, D), dtype=ml_dtypes.bfloat16)
    c["c_piota"] = np.tile(np.arange(NE, dtype=np.float32)[:, None], (1, 128))
    return c


_NC_CACHE = {}


def kernel(**inputs):
    inp = {k: np.asarray(v) for k, v in inputs.items()}
    B = inp["x"].shape[0]
    if "nc" not in _NC_CACHE:
        _NC_CACHE["nc"] = build_two_pass()
    nc = _NC_CACHE["nc"]
    consts = _consts()
    shared = {
        "meta_tokens": inp["meta_tokens"], "ln_emb_g": inp["ln_emb_g"].reshape(1, D), "ln_emb_b": inp["ln_emb_b"].reshape(1, D),
        "w_in": inp["w_in"][0], "lambda_q1": inp["lambda_q1"], "lambda_k1": inp["lambda_k1"],
        "lambda_q2": inp["lambda_q2"], "lambda_k2": inp["lambda_k2"], "subln_g": inp["subln_g"],
        "hgrn_lb_table": inp["hgrn_lb_table"], "hgrn_norm_g": inp["hgrn_norm_g"], "w_out": inp["w_out"][0],
        "ln1_g": inp["ln1_g"], "ln1_b": inp["ln1_b"], "w_router": inp["w_router"][0], "b_router": inp["b_router"],
        "w_gate_up": inp["w_gate_up"][0], "b_gate_up": inp["b_gate_up"][0], "w_down": inp["w_down"][0], "b_down": inp["b_down"][0],
        "ln2_g": inp["ln2_g"], "ln2_b": inp["ln2_b"],
    }
    shared = {k: np.ascontiguousarray(v, dtype=np.float32) for k, v in shared.items()}
    shared.update(consts)
    in_maps = []
    for b in range(B):
        m = dict(shared)
        m["x"] = np.ascontiguousarray(inp["x"][b], dtype=np.float32)
        in_maps.append(m)
    res = run_bass_kernel_spmd(nc, in_maps, core_ids=list(range(B)))
    kernel.last_results = res
    return np.stack([np.asarray(r["out"], dtype=np.float32) for r in res.results], axis=0)
```
